# Optimizing a Trainium2 kernel written in Bass

```python
import math
import jax, jax.numpy as jnp
from jax import lax
import numpy as np

D_MODEL = 2048
BATCH = 8
SEQ = 2048
DEPTH = 2

PLE_DIM = 256
MIX_W = D_MODEL // 2
N_BRANCH = 3
GMLP_GROUPS = 4
GMLP_CHUNK = 128
GMLP_GW = MIX_W // GMLP_GROUPS
POOL_WINDOWS = (2, 4, 8, 16)
POOL_GW = MIX_W // len(POOL_WINDOWS)
ATT_HEADS = 8
HEAD_DIM = MIX_W // ATT_HEADS
MOBA_BLOCK = 256
MOBA_TOPK = 3
Q_BLOCK = 64
D_FF = 4 * D_MODEL
ALPHA = (2.0 * DEPTH) ** 0.25
BETA = (8.0 * DEPTH) ** -0.25
LN_EPS = 1e-5
NEG_INF = -1e30
IN_W = 2 * MIX_W + MIX_W + 3 * MIX_W + N_BRANCH * D_MODEL

kernel_name = 'hybrid_gmlp_pool_moba_deepnorm'


def layer_norm(x, g, b):
    xf = x.astype(jnp.float32)
    mu = jnp.mean(xf, axis=-1, keepdims=True)
    var = jnp.mean(jnp.square(xf - mu), axis=-1, keepdims=True)
    return ((xf - mu) * lax.rsqrt(var + LN_EPS)).astype(x.dtype) * g + b


def alibi_slopes():
    h = jnp.arange(1, ATT_HEADS + 1, dtype=jnp.float32)
    return jnp.exp2(-8.0 * h / ATT_HEADS)


def mixer_gmlp(z, w_sp, b_sp, vn_g, vn_b):
    u, v = jnp.split(jax.nn.gelu(z), 2, axis=-1)
    v = layer_norm(v, vn_g, vn_b)
    bsz, s, _ = v.shape
    nc = s // GMLP_CHUNK
    v = v.reshape(bsz, nc, GMLP_CHUNK, GMLP_GROUPS, GMLP_GW)
    causal = jnp.tril(jnp.ones((GMLP_CHUNK, GMLP_CHUNK), dtype=bool))
    w = jnp.where(causal[None], w_sp, 0.0)
    sv = jnp.einsum('gts,bcsgd->bctgd', w, v) + b_sp.T[None, None, :, :, None]
    return u * sv.reshape(bsz, s, MIX_W)


def mixer_pool(z, w_pool, pool_scale):
    bsz, s, _ = z.shape
    zf = z.astype(jnp.float32)
    cs = lax.cumsum(zf, axis=1)
    outs = []
    for g, w in enumerate(POOL_WINDOWS):
        sl = slice(g * POOL_GW, (g + 1) * POOL_GW)
        c = cs[..., sl]
        lag = jnp.pad(c, ((0, 0), (w, 0), (0, 0)))[:, :s]
        cnt = jnp.minimum(jnp.arange(1, s + 1), w).astype(jnp.float32)[None, :, None]
        outs.append((c - lag) / cnt - zf[..., sl])
    pooled = jnp.stack(outs, axis=2).astype(z.dtype)
    y = jnp.einsum('bsgc,gcd->bsgd', pooled, w_pool).reshape(bsz, s, MIX_W)
    return y * pool_scale


def mixer_moba(q, k, v):
    bsz, s, nh, dh = q.shape
    nb = -(-s // MOBA_BLOCK)
    s_pad = nb * MOBA_BLOCK
    q = jnp.transpose(q, (0, 2, 1, 3)) * (dh ** -0.5)
    pad = ((0, 0), (0, 0), (0, s_pad - s), (0, 0))
    k = jnp.pad(jnp.transpose(k, (0, 2, 1, 3)), pad)
    v = jnp.pad(jnp.transpose(v, (0, 2, 1, 3)), pad)
    kb = k.reshape(bsz, nh, nb, MOBA_BLOCK, dh)
    vb = v.reshape(bsz, nh, nb, MOBA_BLOCK, dh)
    kmean = jnp.mean(kb.astype(jnp.float32), axis=3)
    qblk = jnp.arange(s) // MOBA_BLOCK
    gate = jnp.einsum('bhtd,bhnd->bhtn', q.astype(jnp.float32), kmean)
    past = jnp.arange(nb)[None, :] < qblk[:, None]
    gate = jnp.where(past[None, None], gate, NEG_INF)
    topk = min(MOBA_TOPK, nb)
    _, sel = lax.top_k(gate, topk)
    slopes = alibi_slopes()
    nqb = s // Q_BLOCK
    offs = jnp.arange(MOBA_BLOCK)

    def one_block(args):
        b, i = args
        t0 = i * Q_BLOCK
        q_c = lax.dynamic_slice_in_dim(q[b], t0, Q_BLOCK, axis=1)
        sel_c = lax.dynamic_slice_in_dim(sel[b], t0, Q_BLOCK, axis=1)
        kb_b = kb[b]
        vb_b = vb[b]
        k_sel = jax.vmap(lambda kk, ii: kk[ii])(kb_b, sel_c)
        v_sel = jax.vmap(lambda vv, ii: vv[ii])(vb_b, sel_c)
        j = t0 // MOBA_BLOCK
        k_own = lax.dynamic_index_in_dim(kb_b, j, axis=1, keepdims=False)
        v_own = lax.dynamic_index_in_dim(vb_b, j, axis=1, keepdims=False)
        t = t0 + jnp.arange(Q_BLOCK)
        s_sel = sel_c[..., None] * MOBA_BLOCK + offs
        s_own = j * MOBA_BLOCK + offs
        sc_sel = jnp.einsum('hqd,hqrkd->hqrk', q_c, k_sel).astype(jnp.float32)
        sc_sel = sc_sel - slopes[:, None, None, None] * (t[None, :, None, None] - s_sel).astype(jnp.float32)
        valid = jnp.arange(topk) < jnp.minimum(j, topk)
        sc_sel = jnp.where(valid[None, None, :, None], sc_sel, NEG_INF)
        sc_own = jnp.einsum('hqd,hkd->hqk', q_c, k_own).astype(jnp.float32)
        sc_own = sc_own - slopes[:, None, None] * (t[:, None] - s_own[None, :]).astype(jnp.float32)
        sc_own = jnp.where((s_own[None, :] <= t[:, None])[None], sc_own, NEG_INF)
        sc = jnp.concatenate([sc_sel.reshape(nh, Q_BLOCK, topk * MOBA_BLOCK), sc_own], axis=-1)
        pr = jax.nn.softmax(sc, axis=-1).astype(v.dtype)
        p_sel = pr[..., :topk * MOBA_BLOCK].reshape(nh, Q_BLOCK, topk, MOBA_BLOCK)
        p_own = pr[..., topk * MOBA_BLOCK:]
        return (jnp.einsum('hqrk,hqrkd->hqd', p_sel, v_sel)
                + jnp.einsum('hqk,hkd->hqd', p_own, v_own))

    bi, qi = jnp.meshgrid(jnp.arange(bsz), jnp.arange(nqb), indexing='ij')
    outs = lax.map(one_block, (bi.reshape(-1), qi.reshape(-1)))
    out = outs.reshape(bsz, nqb, nh, Q_BLOCK, dh).transpose(0, 1, 3, 2, 4)
    return out.reshape(bsz, s, nh * dh)


def setup_inputs(seed: int = 0) -> dict:
    key = jax.random.key(seed)
    ks = jax.random.split(key, 20)
    f32 = jnp.float32
    nrm = lambda k, shape, scale: jax.random.normal(k, shape, f32) * scale
    L = DEPTH
    return {
        'x': nrm(ks[0], (BATCH, SEQ, D_MODEL), 1.0),
        'p': nrm(ks[1], (DEPTH, BATCH, SEQ, PLE_DIM), 1.0),
        'w_in': nrm(ks[2], (L, D_MODEL, IN_W), D_MODEL ** -0.5),
        'w_sp': nrm(ks[3], (L, GMLP_GROUPS, GMLP_CHUNK, GMLP_CHUNK), GMLP_CHUNK ** -0.5),
        'b_sp': 1.0 + nrm(ks[4], (L, GMLP_GROUPS, GMLP_CHUNK), 0.02),
        'vn_g': 1.0 + nrm(ks[5], (L, MIX_W), 0.02),
        'vn_b': nrm(ks[6], (L, MIX_W), 0.02),
        'w_pool': nrm(ks[7], (L, len(POOL_WINDOWS), POOL_GW, POOL_GW), POOL_GW ** -0.5),
        'pool_scale': 1.0 + nrm(ks[8], (L, MIX_W), 0.02),
        'w_branch': nrm(ks[9], (L, N_BRANCH, MIX_W, D_MODEL), MIX_W ** -0.5),
        'w_out': nrm(ks[10], (L, D_MODEL, D_MODEL), BETA * D_MODEL ** -0.5),
        'ln1_g': 1.0 + nrm(ks[11], (L, D_MODEL), 0.02),
        'ln1_b': nrm(ks[12], (L, D_MODEL), 0.02),
        'w_up': nrm(ks[13], (L, D_MODEL, D_FF), D_MODEL ** -0.5),
        'w_down': nrm(ks[14], (L, D_FF, D_MODEL), BETA * D_FF ** -0.5),
        'w_ple': nrm(ks[15], (L, PLE_DIM, D_MODEL), PLE_DIM ** -0.5),
        'w_ple_gate': nrm(ks[16], (L, D_MODEL, D_MODEL), D_MODEL ** -0.5),
        'ln2_g': 1.0 + nrm(ks[17], (L, D_MODEL), 0.02),
        'ln2_b': nrm(ks[18], (L, D_MODEL), 0.02),
    }


def reference(x, p, w_in, w_sp, b_sp, vn_g, vn_b, w_pool, pool_scale, w_branch, w_out,
              ln1_g, ln1_b, w_up, w_down, w_ple, w_ple_gate, ln2_g, ln2_b):
    bsz, s, _ = x.shape
    splits = [2 * MIX_W, 3 * MIX_W, 4 * MIX_W, 5 * MIX_W, 6 * MIX_W]
    for i in range(DEPTH):
        h = x @ w_in[i]
        z_a, z_b, z_q, z_k, z_v, z_g = jnp.split(h, splits, axis=-1)
        y_a = mixer_gmlp(z_a, w_sp[i], b_sp[i], vn_g[i], vn_b[i])
        y_b = mixer_pool(z_b, w_pool[i], pool_scale[i])
        hs = (bsz, s, ATT_HEADS, HEAD_DIM)
        y_c = mixer_moba(z_q.reshape(hs), z_k.reshape(hs), z_v.reshape(hs))
        ys = jnp.stack([y_a, y_b, y_c], axis=2)
        branch = jnp.einsum('bsnc,ncd->bsnd', ys, w_branch[i])
        gates = jax.nn.sigmoid(z_g).reshape(bsz, s, N_BRANCH, D_MODEL)
        mixed = jnp.sum(gates * branch, axis=2) @ w_out[i]
        x = layer_norm(ALPHA * x + mixed, ln1_g[i], ln1_b[i])
        ff = jnp.square(jax.nn.relu(x @ w_up[i])) @ w_down[i]
        ple = (p[i] @ w_ple[i]) * jax.nn.sigmoid(x @ w_ple_gate[i])
        x = layer_norm(ALPHA * x + ff + ple, ln2_g[i], ln2_b[i])
    return x
```

```python
import numpy as np
import concourse.bass as bass
import concourse.mybir as mybir
from concourse.bass_utils import run_bass_kernel_spmd

F32 = mybir.dt.float32
BF16 = mybir.dt.bfloat16
AF = mybir.ActivationFunctionType
ALU = mybir.AluOpType
AX = mybir.AxisListType

D = 2048
S = 2048
DEPTH = 2
MIXW = 1024
DFF = 8192
PLE = 256
TT = 512
NT = S // TT
NH = 8
ALPHA = (2.0 * DEPTH) ** 0.25
LN_EPS = 1e-5
NEGM = -30000.0
USZ = 2048
PGSZ = 2304


class Atom:
    __slots__ = ("w", "r", "x")

    def __init__(self, x=False):
        self.w = None
        self.r = {}
        self.x = x


class Buf:
    def __init__(self, t, natoms=1):
        self.t = t
        self.atoms = [Atom() for _ in range(natoms)]

    def a(self, i=None, j=None):
        if i is None:
            return self.atoms
        if j is None:
            return [self.atoms[i]]
        return self.atoms[i:j]


class Chan:
    def __init__(self, sem):
        self.sem = sem
        self.count = 0


class EngState:
    def __init__(self, name, sem):
        self.name = name
        self.sem = sem
        self.count = 0
        self.seen = {}
        self.ops = []


class Sched:
    def __init__(self, nc):
        self.nc = nc
        self.sems = []
        self.eng = {}
        for n in ("pe", "act", "dve", "pool"):
            self.eng[n] = EngState(n, self.new_sem("s_" + n))
        self.eng["sp"] = EngState("sp", None)
        self.chans = []

    def new_sem(self, name):
        h = self.nc.alloc_semaphore(name)
        self.sems.append(h)
        return len(self.sems) - 1

    def chan(self, name):
        c = Chan(self.new_sem("c_" + name))
        self.chans.append(c)
        return c

    def issue(self, engname, fn, reads=(), writes=(), inc=True, chan=None):
        e = self.eng[engname]
        waits = {}

        def need(tok):
            if tok is None:
                return
            s, v = tok
            if engname == "pe" and s == e.sem:
                return
            if waits.get(s, 0) < v:
                waits[s] = v

        reads = list(reads)
        writes = list(writes)
        xr = [a for a in reads if a.x]
        if xr:
            reads = [a for a in reads if not a.x]
            writes = writes + [a for a in xr if a not in writes]
        for a in reads:
            need(a.w)
        for a in writes:
            need(a.w)
            for s, v in a.r.items():
                need((s, v))
        wl = []
        for s, v in waits.items():
            if e.seen.get(s, 0) < v:
                e.seen[s] = v
                wl.append((s, v))
        if engname == "sp":
            chan.count += 16
            tok = (chan.sem, chan.count)
            incinfo = (chan.sem, 16)
        else:
            if inc:
                e.count += 1
                tok = (e.sem, e.count)
                incinfo = (e.sem, 1)
            else:
                tok = (e.sem, e.count + 1)
                incinfo = None
        e.ops.append((wl, fn, incinfo))
        for a in reads:
            if a.r.get(tok[0], 0) < tok[1]:
                a.r[tok[0]] = tok[1]
        for a in writes:
            a.w = tok
            a.r = {}

    def emit(self, engname, engine):
        e = self.eng[engname]
        for wl, fn, incinfo in e.ops:
            for s, v in wl:
                engine.wait_ge(self.sems[s], v)
            ins = fn(engine)
            if incinfo is not None:
                ins.then_inc(self.sems[incinfo[0]], incinfo[1])
        if engname == "sp":
            for c in self.chans:
                if c.count > 0:
                    engine.wait_ge(self.sems[c.sem], c.count)


def unit_list():
    u = []
    for c in range(8):
        u.append(("gv", c))
    for c in range(8):
        u.append(("gu", c))

    def branch(n):
        for dcp in range(8):
            u.append(("gate", n, 2 * dcp))
            u.append(("gate", n, 2 * dcp + 1))
            u.append(("br", n, dcp))

    branch(0)
    for c in range(8):
        u.append(("pz", c))
    u.append(("pw",))
    branch(1)
    for c in range(8):
        u.append(("q", c))
    for c in range(8):
        u.append(("k", c))
    for c in range(8):
        u.append(("av", c))
    branch(2)
    for dc in range(16):
        u.append(("wo", dc))
    for fg in range(4):
        for fc in range(16):
            u.append(("up", fg, fc))
        for dc in range(16):
            u.append(("dn", fg, dc))
    for dc in range(16):
        u.append(("pg", dc))
    return u


UNITS = unit_list()
UOFF = []
_o = 0
for _u in UNITS:
    UOFF.append(_o)
    _o += PGSZ if _u[0] == "pg" else USZ
WTOT = _o


def _kunit(W):
    K = W.shape[0]
    return np.ascontiguousarray(W.reshape(K // 128, 128, 128).transpose(1, 0, 2)).reshape(128, K)


def build_wstream(w_in, w_pool, w_branch, w_out, w_up, w_down, w_ple, w_ple_gate):
    out = np.empty((128, WTOT), dtype=np.float32)
    win_c = lambda c: w_in[:, c * 128:(c + 1) * 128]
    for ui, u in enumerate(UNITS):
        o = UOFF[ui]
        k = u[0]
        if k == "gv":
            a = _kunit(win_c(8 + u[1]))
        elif k == "gu":
            a = _kunit(win_c(u[1]))
        elif k == "gate":
            a = _kunit(win_c(48 + u[1] * 16 + u[2]))
        elif k == "br":
            n, dcp = u[1], u[2]
            a = np.concatenate([_kunit(w_branch[n][:, (2 * dcp + j) * 128:(2 * dcp + j + 1) * 128]) for j in range(2)], axis=1)
        elif k == "pz":
            a = _kunit(win_c(16 + u[1]))
        elif k == "pw":
            a = np.ascontiguousarray(w_pool.reshape(4, 2, 128, 256).transpose(2, 0, 1, 3)).reshape(128, 2048)
        elif k == "q":
            a = _kunit(win_c(24 + u[1]))
        elif k == "k":
            a = _kunit(win_c(32 + u[1]))
        elif k == "av":
            a = _kunit(win_c(40 + u[1]))
        elif k == "wo":
            a = _kunit(w_out[:, u[1] * 128:(u[1] + 1) * 128])
        elif k == "up":
            c = u[1] * 16 + u[2]
            a = _kunit(w_up[:, c * 128:(c + 1) * 128])
        elif k == "dn":
            fg, dc = u[1], u[2]
            a = _kunit(w_down[fg * 2048:(fg + 1) * 2048, dc * 128:(dc + 1) * 128])
        elif k == "pg":
            dc = u[1]
            a = np.concatenate([_kunit(w_ple_gate[:, dc * 128:(dc + 1) * 128]),
                                _kunit(w_ple[:, dc * 128:(dc + 1) * 128])], axis=1)
        out[:, o:o + a.shape[1]] = a
    return out


def build_consts():
    c = {}
    c["ident"] = np.eye(128, dtype=np.float32)
    c["ones"] = np.ones((128, 128), dtype=np.float32)
    ind = np.zeros((128, 64), dtype=np.float32)
    for k in range(128):
        ind[k, k % 64] = 1.0
    c["ind"] = ind
    s = np.arange(128)[:, None]
    t = np.arange(128)[None, :]
    tri = np.where(s <= t, 0.0, NEGM).astype(np.float32)
    cm = np.zeros((128, 2, 256), dtype=np.float32)
    cm[:, 0, 0:128] = tri
    cm[:, 0, 128:256] = 0.0
    cm[:, 1, 0:128] = NEGM
    cm[:, 1, 128:256] = tri
    c["cmask"] = cm
    slopes = np.exp2(-8.0 * np.arange(1, 9, dtype=np.float64) / 8.0)
    ak = np.zeros((128, 8, 16), dtype=np.float64)
    for idx in range(16):
        ak[:, :, idx] = (np.arange(128)[:, None] - (idx - 1) * 128) * slopes[None, :]
    c["akey"] = ak.astype(np.float32)
    pn = np.zeros((128, 8, 8), dtype=np.float32)
    no = np.ones((128, 8, 8), dtype=np.float32)
    for qb in range(8):
        for n in range(8):
            if n >= qb:
                pn[:, qb, n] = -1e30
            if n == qb:
                no[:, qb, n] = 0.0
    c["pastneg"] = pn
    c["notown"] = no
    aq = np.zeros((128, 2, 8, 8), dtype=np.float64)
    for par in range(2):
        aq[:, par, :, :] = (-(np.arange(128)[:, None] + 128 * par) * slopes[None, :])[:, :, None]
    c["aq"] = aq.reshape(128, 2, 64).astype(np.float32)
    c["spmask"] = np.where(s <= t, 1.0, 0.0).astype(np.float32)
    ic = np.zeros((128, 4, 16), dtype=np.float32)
    for g, w in enumerate((2, 4, 8, 16)):
        ic[:, g, :] = 1.0 / np.minimum(np.arange(1, 17), w)
    c["invcnt"] = ic
    return c


CONST_SHAPES = {
    "ident": [128, 128], "ones": [128, 128], "ind": [128, 64], "cmask": [128, 2, 256],
    "akey": [128, 8, 16], "pastneg": [128, 8, 8], "notown": [128, 8, 8], "aq": [128, 2, 64],
    "spmask": [128, 128], "invcnt": [128, 4, 16],
}
BF_CONSTS = ("ident", "ones", "ind", "cmask")
LCONST_SHAPES = {
    "ln1g": [128, 16], "ln1b": [128, 16], "ln2g": [128, 16], "ln2b": [128, 16],
    "pscale": [128, 8], "vng": [128, 1024], "vnb": [128, 1024], "bsp": [128, 4, 128],
    "wspT": [128, 4, 128],
}
L_VIA_TMP = ("vng", "vnb", "wspT")


def build_program(n_layers=DEPTH, n_tiles=NT, stop_stage=99):
    nc = bass.Bass("TRN2", target_bir_lowering=False)
    sc = Sched(nc)

    def dram(name, shape, kind):
        return nc.dram_tensor(name, shape, F32, kind=kind).ap()

    xT_d = dram("xT", [D, S], "ExternalInput")
    pT_d = dram("pT", [DEPTH, PLE, S], "ExternalInput")
    ws_d = dram("wstream", [DEPTH, 128, WTOT], "ExternalInput")
    cd = {k: dram("c_" + k, v, "ExternalInput") for k, v in CONST_SHAPES.items()}
    lcd = {k: dram("l_" + k, [DEPTH] + v, "ExternalInput") for k, v in LCONST_SHAPES.items()}
    out_d = dram("outT", [D, S], "ExternalOutput")
    scr_d = dram("xscr", [D, S], "Internal") if n_layers > 1 else None
    scr_atoms = [[Atom() for _ in range(16)] for _ in range(NT)]

    def sb(name, shape, dt, natoms=1):
        return Buf(nc.alloc_sbuf_tensor(name, shape, dt), natoms)

    def ps(name, natoms=1):
        b = Buf(nc.alloc_psum_tensor(name, [128, 512], F32), natoms)
        for a in b.atoms:
            a.x = True
        return b

    kT = sb("kT", [128, NH, S], BF16, 16)
    Vh = sb("Vh", [128, 16, MIXW], BF16, 16)
    kmean = sb("kmean", [128, NH, 8], BF16, 1)
    R = sb("R", [128, 16, TT], F32, 16)
    XB = [sb("XB0", [128, 16, TT], BF16, 16), sb("XB1", [128, 16, TT], BF16, 16)]
    MIXF = sb("MIXF", [128, 16, TT], BF16, 16)
    NSTG, NWB, NTMP = 2, 2, 6
    NW = TT + 16
    stg = [sb("stg%d" % i, [128, PGSZ], F32) for i in range(NSTG)]
    wbs = [sb("wb%d" % i, [128, PGSZ], BF16) for i in range(NWB)]
    tmps = [sb("tmp%d" % i, [128, NW], F32) for i in range(NTMP)]
    halo = sb("halo", [128, 8, 16], F32, 8)
    pTb = sb("pTb", [128, 2, TT], BF16)
    cT = sb("cT", [128, TT], BF16, 4)
    PTs = [sb("PT%d" % i, [128, 256], BF16) for i in range(3)]
    small = {k: sb("sm_" + k, shp, dt) for k, shp, dt in [
        ("gm", [128, 64], F32), ("max8", [128, 8, 8], F32), ("cf", [128, 64], F32),
        ("c2", [128, 64], F32), ("chl", [128, 128], BF16), ("ksum", [128, 2], F32),
        ("bst", [128, 2, 6], F32), ("mv", [128, 2], F32), ("rstd", [128, 1], F32),
    ]}
    cb = {}
    for k, shp in CONST_SHAPES.items():
        cb[k] = sb("cb_" + k, shp, BF16 if k in BF_CONSTS else F32)
    lcb = {}
    for k, shp in LCONST_SHAPES.items():
        if k == "wspT":
            continue
        lcb[k] = sb("lcb_" + k, shp, BF16 if k in L_VIA_TMP else F32)
    wspb = sb("wspb", [128, 4, 128], BF16)
    mmps = [ps("mm%d" % i) for i in range(4)]
    Sps = [ps("S%d" % i) for i in range(2)]
    OSps = [ps("OS%d" % i) for i in range(2)]

    ch_stg = [sc.chan("stg%d" % i) for i in range(NSTG)]
    ch_tmp = [sc.chan("tmp%d" % i) for i in range(NTMP)]
    ch_R = [sc.chan("R%d" % i) for i in range(16)]

    state = {"tmp": 0, "mm": 0, "stg": 0, "wb": 0, "S": 0, "PT": 0}

    def next_tmp():
        i = state["tmp"]
        state["tmp"] = (i + 1) % NTMP
        return tmps[i], ch_tmp[i]

    def T(tb):
        return tb.t[:, 0:TT]

    def TB(tb):
        return tb.t[:].bitcast(BF16)[:, 0:TT]

    def T4(tb):
        return tb.t[:, 0:TT].rearrange("p (a b) -> p a b", a=4)

    def next_mm():
        i = state["mm"]
        state["mm"] = (i + 1) % 4
        return mmps[i]

    def pe(fn, reads, writes, inc=False):
        sc.issue("pe", fn, reads, writes, inc=inc)

    def act(fn, reads, writes):
        sc.issue("act", fn, reads, writes)

    def dve(fn, reads, writes):
        sc.issue("dve", fn, reads, writes)

    def pool(fn, reads, writes):
        sc.issue("pool", fn, reads, writes)

    def dma(fn, reads, writes, chan):
        sc.issue("sp", fn, reads, writes, chan=chan)

    def flat(ap, nd):
        return ap.rearrange("p a b -> p (a b)") if nd == 3 else ap

    def load_via_tmp(dst_ap, dst_atoms, src_ap, n, mul_ap=None, mul_atoms=()):
        for o in range(0, n, TT):
            m = min(TT, n - o)
            tb, tch = next_tmp()
            dma(lambda e, tb=tb, o=o, m=m: e.dma_start(out=tb.t[:, 0:m], in_=src_ap[:, o:o + m]), [], tb.a(), tch)
            if mul_ap is None:
                dve(lambda e, tb=tb, o=o, m=m: e.tensor_copy(out=dst_ap[:, o:o + m], in_=tb.t[:, 0:m]), tb.a(), dst_atoms)
            else:
                dve(lambda e, tb=tb, o=o, m=m: e.tensor_tensor(out=dst_ap[:, o:o + m].rearrange("p (a b) -> p a b", a=4),
                                                               in0=tb.t[:, 0:m].rearrange("p (a b) -> p a b", a=4),
                                                               in1=mul_ap, op=ALU.mult), tb.a() + list(mul_atoms), dst_atoms)

    for k, shp in CONST_SHAPES.items():
        if k in BF_CONSTS:
            load_via_tmp(flat(cb[k].t[:], len(shp)), cb[k].a(), flat(cd[k], len(shp)), int(np.prod(shp[1:])))
        else:
            ch = sc.chan("cb_" + k)
            dma(lambda e, k=k: e.dma_start(out=cb[k].t[:], in_=cd[k]), [], cb[k].a(), ch)
    dve(lambda e: e.memset(kmean.t[:], 0.0), [], kmean.a())
    lchan = {k: sc.chan("lcb_" + k) for k in LCONST_SHAPES if k not in L_VIA_TMP}

    def load_unit(l, ui):
        size = PGSZ if UNITS[ui][0] == "pg" else USZ
        off = UOFF[ui]
        si = state["stg"]
        state["stg"] = (si + 1) % NSTG
        wi = state["wb"]
        state["wb"] = (wi + 1) % NWB
        sg, wb = stg[si], wbs[wi]
        dma(lambda e: e.dma_start(out=sg.t[:, 0:size], in_=ws_d[l, :, off:off + size]), [], sg.a(), ch_stg[si])
        pool(lambda e: e.tensor_copy(out=wb.t[:, 0:size], in_=sg.t[:, 0:size]), sg.a(), wb.a())
        return wb

    def mm_f1(wb, woff, nk, act_fn, act_atoms, psb):
        for kc in range(nk):
            pe(lambda e, kc=kc: e.matmul(psb.t[:], lhsT=wb.t[:, woff + kc * 128: woff + (kc + 1) * 128],
                                         rhs=act_fn(kc), start=(kc == 0), stop=(kc == nk - 1)),
               wb.a() + act_atoms, psb.a(), inc=(kc == nk - 1))

    def mm_f2(wb, xb, psb):
        for tc in range(4):
            for kc in range(16):
                pe(lambda e, tc=tc, kc=kc: e.matmul(psb.t[:, tc * 128:(tc + 1) * 128],
                                                    lhsT=xb.t[:, kc, tc * 128:(tc + 1) * 128],
                                                    rhs=wb.t[:, kc * 128:(kc + 1) * 128],
                                                    start=(kc == 0), stop=(kc == 15)),
                   wb.a() + xb.a(), psb.a(), inc=(tc == 3 and kc == 15))

    GC = 1.5957691216057308

    def gelu_from_psum(psb, out_ap_fn, out_atoms, view3=False):
        t1, _ = next_tmp()
        t2, _ = next_tmp()
        act(lambda e: e.activation(out=T(t1), in_=psb.t[:], func=AF.Identity), psb.a(), t1.a())
        dve(lambda e: e.tensor_tensor(out=T(t2), in0=T(t1), in1=T(t1), op=ALU.mult), t1.a(), t2.a())
        dve(lambda e: e.tensor_scalar(out=T(t2), in0=T(t2), scalar1=0.044715, scalar2=1.0, op0=ALU.mult, op1=ALU.add),
            t2.a(), t2.a())
        dve(lambda e: e.tensor_tensor(out=T(t2), in0=T(t2), in1=T(t1), op=ALU.mult), t1.a() + t2.a(), t2.a())
        act(lambda e: e.activation(out=T(t2), in_=T(t2), func=AF.Sigmoid, scale=GC), t2.a(), t2.a())
        if view3:
            dve(lambda e: e.tensor_tensor(out=out_ap_fn(), in0=T4(t2), in1=T4(t1), op=ALU.mult), t1.a() + t2.a(), out_atoms)
        else:
            dve(lambda e: e.tensor_tensor(out=out_ap_fn(), in0=T(t2), in1=T(t1), op=ALU.mult), t1.a() + t2.a(), out_atoms)

    ones_bf = cb["ones"]

    def mixf_f32(i):
        return MIXF.t[:, 2 * i:2 * i + 2, :].rearrange("p a b -> p (a b)").bitcast(F32)

    def layer_norm_R(gk, bk, xb_out):
        ps_sum = next_mm()
        ps_sq = next_mm()
        for dc in range(16):
            t1, _ = next_tmp()
            act(lambda e, dc=dc, t1=t1: e.activation(out=TB(t1), in_=R.t[:, dc, :], func=AF.Identity), R.a(dc), t1.a())
            t2, _ = next_tmp()
            act(lambda e, dc=dc, t2=t2: e.activation(out=TB(t2), in_=R.t[:, dc, :], func=AF.Square), R.a(dc), t2.a())
            pe(lambda e, dc=dc, t1=t1: e.matmul(ps_sum.t[:], lhsT=ones_bf.t[:], rhs=TB(t1), start=(dc == 0), stop=(dc == 15)),
               t1.a() + ones_bf.a(), ps_sum.a(), inc=True)
            pe(lambda e, dc=dc, t2=t2: e.matmul(ps_sq.t[:], lhsT=ones_bf.t[:], rhs=TB(t2), start=(dc == 0), stop=(dc == 15)),
               t2.a() + ones_bf.a(), ps_sq.a(), inc=True)
        M, A, B = mixf_f32(0), mixf_f32(1), mixf_f32(2)
        Ma, Aa, Ba = MIXF.a(0, 2), MIXF.a(2, 4), MIXF.a(4, 6)
        dve(lambda e: e.tensor_scalar(out=M, in0=ps_sum.t[:], scalar1=1.0 / D, scalar2=None, op0=ALU.mult), ps_sum.a(), Ma)
        dve(lambda e: e.tensor_tensor(out=B, in0=M, in1=M, op=ALU.mult), Ma, Ba)
        dve(lambda e: e.scalar_tensor_tensor(out=A, in0=ps_sq.t[:], scalar=1.0 / D, in1=B, op0=ALU.mult, op1=ALU.subtract),
            ps_sq.a() + Ba, Aa)
        dve(lambda e: e.tensor_scalar(out=A, in0=A, scalar1=LN_EPS, scalar2=None, op0=ALU.add), Aa, Aa)
        act(lambda e: e.activation(out=A, in_=A, func=AF.Sqrt), Aa, Aa)
        dve(lambda e: e.reciprocal(out=A, in_=A), Aa, Aa)
        dve(lambda e: e.scalar_tensor_tensor(out=B, in0=M, scalar=-1.0, in1=A, op0=ALU.mult, op1=ALU.mult), Ma + Aa, Ba)
        for dc in range(16):
            t1, _ = next_tmp()
            dve(lambda e, dc=dc, t1=t1: e.tensor_tensor(out=T(t1), in0=R.t[:, dc, :], in1=A, op=ALU.mult), R.a(dc) + Aa, t1.a())
            dve(lambda e, t1=t1: e.tensor_tensor(out=T(t1), in0=T(t1), in1=B, op=ALU.add), t1.a() + Ba, t1.a())
            dve(lambda e, dc=dc, t1=t1: e.tensor_scalar(out=R.t[:, dc, :], in0=T(t1), scalar1=lcb[gk].t[:, dc:dc + 1],
                                                        scalar2=lcb[bk].t[:, dc:dc + 1], op0=ALU.mult, op1=ALU.add),
                t1.a() + lcb[gk].a() + lcb[bk].a(), R.a(dc))
            if xb_out is not None:
                act(lambda e, dc=dc: e.activation(out=xb_out.t[:, dc, :], in_=R.t[:, dc, :], func=AF.Identity), R.a(dc), xb_out.a(dc))

    def load_xbf(l, ti, xb):
        src = xT_d if l == 0 else scr_d
        for dc in range(16):
            tb, tch = next_tmp()
            rd = [] if l == 0 else [scr_atoms[ti][dc]]
            dma(lambda e, dc=dc, tb=tb: e.dma_start(out=T(tb), in_=src[dc * 128:(dc + 1) * 128, ti * TT:(ti + 1) * TT]),
                rd, tb.a(), tch)
            dve(lambda e, dc=dc, tb=tb: e.tensor_copy(out=xb.t[:, dc, :], in_=T(tb)), tb.a(), xb.a(dc))

    def R_bf(c0, n):
        return R.t[:, c0:c0 + n, :].rearrange("p a b -> p (a b)").bitcast(BF16)

    for l in range(n_layers):
        for k in LCONST_SHAPES:
            if k in ("vng", "vnb"):
                load_via_tmp(lcb[k].t[:], lcb[k].a(), lcd[k][l], 1024)
            elif k == "wspT":
                load_via_tmp(wspb.t[:].rearrange("p a b -> p (a b)"), wspb.a(), lcd[k][l].rearrange("p a b -> p (a b)"), 512,
                             mul_ap=cb["spmask"].t[:].unsqueeze(1).to_broadcast([128, 4, 128]), mul_atoms=cb["spmask"].a())
            else:
                dma(lambda e, k=k, l=l: e.dma_start(out=lcb[k].t[:], in_=lcd[k][l]), [], lcb[k].a(), lchan[k])
        dve(lambda e: e.memset(halo.t[:], 0.0), [], halo.a())
        load_xbf(l, 0, XB[0])

        def tile_body(l, ti):
            xb = XB[0]
            xb1 = XB[1]
            ui = [0]

            def finish():
                for dc in range(16):
                    dma(lambda e, dc=dc: e.dma_start(out=out_d[dc * 128:(dc + 1) * 128, ti * TT:(ti + 1) * TT], in_=R.t[:, dc, :]),
                        R.a(dc), [], ch_R[dc])

            def nxt():
                w = load_unit(l, ui[0])
                ui[0] += 1
                return w

            xact = lambda kc: xb.t[:, kc, :]
            x1act = lambda kc: xb1.t[:, kc, :]
            mixact = lambda kc: MIXF.t[:, kc, :]

            for kc in range(2):
                tb, tch = next_tmp()
                dma(lambda e, kc=kc, tb=tb: e.dma_start(out=T(tb), in_=pT_d[l, kc * 128:(kc + 1) * 128, ti * TT:(ti + 1) * TT]),
                    [], tb.a(), tch)
                dve(lambda e, kc=kc, tb=tb: e.tensor_copy(out=pTb.t[:, kc, :], in_=T(tb)), tb.a(), pTb.a())

            vtok = R.t[:, 0:8, :].rearrange("p a b -> p (a b)").rearrange("p (t c) -> p t c", t=4)
            vbf = R_bf(8, 4).rearrange("p (t c) -> p t c", t=4)
            for c in range(8):
                wb = nxt()
                psb = next_mm()
                mm_f2(wb, xb, psb)
                gelu_from_psum(psb, lambda c=c: vtok[:, :, c * 128:(c + 1) * 128], R.a(0, 8), view3=True)
            bst, mv, rstd = small["bst"], small["mv"], small["rstd"]
            for tc in range(4):
                for hh in range(2):
                    dve(lambda e, tc=tc, hh=hh: e.bn_stats(out=bst.t[:, hh, :], in_=vtok[:, tc, hh * 512:(hh + 1) * 512]), R.a(0, 8), bst.a())
                dve(lambda e: e.bn_aggr(out=mv.t[:], in_=bst.t[:].rearrange("p a b -> p (a b)")), bst.a(), mv.a())
                dve(lambda e: e.tensor_scalar(out=rstd.t[:], in0=mv.t[:, 1:2], scalar1=LN_EPS, scalar2=None, op0=ALU.add), mv.a(), rstd.a())
                act(lambda e: e.activation(out=rstd.t[:], in_=rstd.t[:], func=AF.Sqrt), rstd.a(), rstd.a())
                dve(lambda e: e.reciprocal(out=rstd.t[:], in_=rstd.t[:]), rstd.a(), rstd.a())
                dve(lambda e, tc=tc: e.tensor_scalar(out=vtok[:, tc, :], in0=vtok[:, tc, :], scalar1=mv.t[:, 0:1], scalar2=rstd.t[:, 0:1],
                                                     op0=ALU.subtract, op1=ALU.mult), R.a(0, 8) + mv.a() + rstd.a(), R.a(0, 8))
                dve(lambda e, tc=tc: e.tensor_tensor(out=vtok[:, tc, :], in0=vtok[:, tc, :], in1=lcb["vng"].t[:], op=ALU.mult),
                    R.a(0, 8) + lcb["vng"].a(), R.a(0, 8))
                dve(lambda e, tc=tc: e.tensor_tensor(out=vbf[:, tc, :], in0=vtok[:, tc, :], in1=lcb["vnb"].t[:], op=ALU.add),
                    R.a(0, 8) + lcb["vnb"].a(), R.a(8, 12))

            if stop_stage == 1:
                finish()
                return
            ybf = R_bf(12, 4).rearrange("p (c t) -> p c t", c=8)
            Y_ATOMS = R.a(12, 16)
            yact = lambda kc: ybf[:, kc, :]

            for c in range(8):
                wb = nxt()
                psb = next_mm()
                mm_f1(wb, 0, 16, xact, xb.a(), psb)
                tu, _ = next_tmp()
                gelu_from_psum(psb, lambda tu=tu: T(tu), tu.a())
                ps2 = next_mm()
                g = c // 2
                for tc in range(4):
                    pe(lambda e, tc=tc, c=c, g=g, ps2=ps2: e.matmul(ps2.t[:, tc * 128:(tc + 1) * 128], lhsT=vbf[:, tc, c * 128:(c + 1) * 128],
                                                                    rhs=wspb.t[:, g, :], start=True, stop=True),
                       R.a(8, 12) + wspb.a(), ps2.a(), inc=(tc == 3))
                t3, _ = next_tmp()
                dve(lambda e, g=g, ps2=ps2, t3=t3: e.tensor_tensor(out=T4(t3), in0=ps2.t[:].rearrange("p (a b) -> p a b", a=4),
                                                                   in1=lcb["bsp"].t[:, g, :].unsqueeze(1).to_broadcast([128, 4, 128]), op=ALU.add),
                    ps2.a() + lcb["bsp"].a(), t3.a())
                dve(lambda e, c=c, t3=t3, tu=tu: e.tensor_tensor(out=ybf[:, c, :], in0=T(t3), in1=T(tu), op=ALU.mult),
                    t3.a() + tu.a(), Y_ATOMS)

            if stop_stage == 2:
                finish()
                return
            def do_branch(n):
                for dcp in range(8):
                    wg0 = nxt()
                    pg0 = next_mm()
                    mm_f1(wg0, 0, 16, xact, xb.a(), pg0)
                    sg0, _ = next_tmp()
                    act(lambda e, pg0=pg0, sg0=sg0: e.activation(out=T(sg0), in_=pg0.t[:], func=AF.Sigmoid), pg0.a(), sg0.a())
                    wg1 = nxt()
                    pg1 = next_mm()
                    mm_f1(wg1, 0, 16, xact, xb.a(), pg1)
                    sg1, _ = next_tmp()
                    act(lambda e, pg1=pg1, sg1=sg1: e.activation(out=T(sg1), in_=pg1.t[:], func=AF.Sigmoid), pg1.a(), sg1.a())
                    wbr = nxt()
                    for j, sg in ((0, sg0), (1, sg1)):
                        dc = 2 * dcp + j
                        pb = next_mm()
                        mm_f1(wbr, j * 1024, 8, yact, Y_ATOMS, pb)
                        if n == 0:
                            dve(lambda e, dc=dc, pb=pb, sg=sg: e.tensor_tensor(out=MIXF.t[:, dc, :], in0=pb.t[:], in1=T(sg), op=ALU.mult),
                                pb.a() + sg.a(), MIXF.a(dc))
                        else:
                            dve(lambda e, pb=pb, sg=sg: e.tensor_tensor(out=T(sg), in0=pb.t[:], in1=T(sg), op=ALU.mult),
                                pb.a() + sg.a(), sg.a())
                            dve(lambda e, dc=dc, sg=sg: e.tensor_tensor(out=MIXF.t[:, dc, :], in0=T(sg), in1=MIXF.t[:, dc, :], op=ALU.add),
                                sg.a() + MIXF.a(dc), MIXF.a(dc))

            do_branch(0)

            if stop_stage == 3:
                finish()
                return
            pooled = R_bf(0, 4).rearrange("p (c t) -> p c t", c=8)
            P_ATOMS = R.a(0, 4)
            for c in range(8):
                wb = nxt()
                psb = next_mm()
                mm_f1(wb, 0, 16, xact, xb.a(), psb)
                zc, _ = next_tmp()
                g = c // 2
                w = 2 << g
                act(lambda e, zc=zc, psb=psb: e.activation(out=zc.t[:, 16:NW], in_=psb.t[:], func=AF.Identity), psb.a(), zc.a())
                dve(lambda e, zc=zc, c=c: e.tensor_copy(out=zc.t[:, 0:16], in_=halo.t[:, c, :]), halo.a(c) + zc.a(), zc.a())
                src = zc
                sh = 1
                while sh < w:
                    dst, _ = next_tmp()
                    dve(lambda e, src=src, dst=dst, sh=sh: e.tensor_tensor(out=dst.t[:, sh:NW], in0=src.t[:, sh:NW], in1=src.t[:, 0:NW - sh], op=ALU.add),
                        src.a(), dst.a())
                    src = dst
                    sh *= 2
                dve(lambda e, src=src, zc=zc, c=c, w=w: e.scalar_tensor_tensor(out=pooled[:, c, :], in0=src.t[:, 16:NW], scalar=1.0 / w,
                                                                               in1=zc.t[:, 16:NW], op0=ALU.mult, op1=ALU.subtract),
                    src.a() + zc.a(), P_ATOMS)
                if ti == 0:
                    t1, _ = next_tmp()
                    dve(lambda e, src=src, g=g, t1=t1: e.tensor_tensor(out=t1.t[:, 0:16], in0=src.t[:, 16:32], in1=cb["invcnt"].t[:, g, :], op=ALU.mult),
                        src.a() + cb["invcnt"].a(), t1.a())
                    dve(lambda e, zc=zc, c=c, t1=t1: e.tensor_tensor(out=pooled[:, c, 0:16], in0=t1.t[:, 0:16], in1=zc.t[:, 16:32], op=ALU.subtract),
                        t1.a() + zc.a(), P_ATOMS)
                dve(lambda e, zc=zc, c=c: e.tensor_copy(out=halo.t[:, c, :], in_=zc.t[:, TT:NW]), zc.a(), halo.a(c))
            wb = nxt()
            for g in range(4):
                for hf in range(2):
                    pb = next_mm()
                    for kc in range(2):
                        o = g * 512 + kc * 256 + hf * 128
                        pe(lambda e, o=o, g=g, kc=kc, pb=pb, wb=wb: e.matmul(pb.t[:], lhsT=wb.t[:, o:o + 128], rhs=pooled[:, 2 * g + kc, :],
                                                                             start=(kc == 0), stop=(kc == 1)),
                           wb.a() + P_ATOMS, pb.a(), inc=(kc == 1))
                    cc = 2 * g + hf
                    dve(lambda e, cc=cc, pb=pb: e.tensor_scalar(out=ybf[:, cc, :], in0=pb.t[:], scalar1=lcb["pscale"].t[:, cc:cc + 1], scalar2=None, op0=ALU.mult),
                        pb.a() + lcb["pscale"].a(), Y_ATOMS)
            if stop_stage == 4:
                finish()
                return
            do_branch(1)

            qT = R_bf(0, 4).rearrange("p (c t) -> p c t", c=8)
            Q_ATOMS = R.a(0, 4)
            for h in range(8):
                wb = nxt()
                psb = next_mm()
                mm_f1(wb, 0, 16, xact, xb.a(), psb)
                act(lambda e, h=h, psb=psb: e.activation(out=qT[:, h, :], in_=psb.t[:], func=AF.Identity, scale=float(128 ** -0.5)), psb.a(), Q_ATOMS)
            ks = small["ksum"]
            for h in range(8):
                wb = nxt()
                psb = next_mm()
                mm_f1(wb, 0, 16, xact, xb.a(), psb)
                act(lambda e, h=h, psb=psb: e.activation(out=kT.t[:, h, ti * TT:(ti + 1) * TT], in_=psb.t[:], func=AF.Identity),
                    psb.a(), kT.a(4 * ti, 4 * ti + 4))
                dve(lambda e, psb=psb: e.tensor_reduce(out=ks.t[:], in_=psb.t[:].rearrange("p (a b) -> p a b", a=2), axis=AX.X, op=ALU.add), psb.a(), ks.a())
                dve(lambda e, h=h: e.tensor_scalar(out=kmean.t[:, h, 2 * ti:2 * ti + 2], in0=ks.t[:], scalar1=1.0 / 256, scalar2=None, op0=ALU.mult),
                    ks.a(), kmean.a())
            for h in range(8):
                wb = nxt()
                psb = next_mm()
                mm_f2(wb, xb, psb)
                act(lambda e, h=h, psb=psb: e.activation(out=Vh.t[:, 4 * ti:4 * ti + 4, h * 128:(h + 1) * 128],
                                                         in_=psb.t[:].rearrange("p (a b) -> p a b", a=4), func=AF.Identity),
                    psb.a(), Vh.a(4 * ti, 4 * ti + 4))

            if stop_stage == 5:
                finish()
                return
            gm, max8, cf, c2, chl = small["gm"], small["max8"], small["cf"], small["c2"], small["chl"]
            v88 = lambda ap: ap.rearrange("p (a b) -> p a b", a=8)
            for tc in range(4):
                qblk = (ti * TT + tc * 128) // 256
                psg = next_mm()
                for h in range(8):
                    pe(lambda e, h=h, tc=tc, psg=psg: e.matmul(psg.t[:, h * 8:(h + 1) * 8], lhsT=qT[:, h, tc * 128:(tc + 1) * 128], rhs=kmean.t[:, h, :],
                                                               start=True, stop=True), Q_ATOMS + kmean.a(), psg.a(), inc=(h == 7))
                dve(lambda e, psg=psg, qblk=qblk: e.tensor_tensor(out=v88(gm.t[:]), in0=v88(psg.t[:, 0:64]),
                                                                  in1=cb["pastneg"].t[:, qblk, :].unsqueeze(1).to_broadcast([128, 8, 8]), op=ALU.add),
                    psg.a() + cb["pastneg"].a(), gm.a())
                for h in range(8):
                    dve(lambda e, h=h: e.max(out=max8.t[:, h, :], in_=gm.t[:, h * 8:(h + 1) * 8]), gm.a(), max8.a())
                for h in range(8):
                    dve(lambda e, h=h: e.tensor_scalar(out=cf.t[:, h * 8:(h + 1) * 8], in0=gm.t[:, h * 8:(h + 1) * 8], scalar1=max8.t[:, h, 2:3],
                                                       scalar2=NEGM, op0=ALU.is_lt, op1=ALU.mult), gm.a() + max8.a(), cf.a())
                dve(lambda e, qblk=qblk: e.tensor_tensor(out=v88(cf.t[:]), in0=v88(cf.t[:]),
                                                         in1=cb["notown"].t[:, qblk, :].unsqueeze(1).to_broadcast([128, 8, 8]), op=ALU.mult),
                    cf.a() + cb["notown"].a(), cf.a())
                dve(lambda e, tc=tc: e.tensor_tensor(out=c2.t[:], in0=cf.t[:], in1=cb["aq"].t[:, tc % 2, :], op=ALU.add), cf.a() + cb["aq"].a(), c2.a())
                dve(lambda e: e.tensor_copy(out=chl.t[:, 0:64], in_=c2.t[:]), c2.a(), chl.a())
                dve(lambda e: e.tensor_tensor(out=chl.t[:, 64:128], in0=c2.t[:], in1=chl.t[:, 0:64], op=ALU.subtract), c2.a() + chl.a(), chl.a())
                pst = next_mm()
                pe(lambda e, pst=pst: e.transpose(out=pst.t[:].bitcast(BF16)[:, 0:128], in_=chl.t[:], identity=cb["ident"].t[:]),
                   chl.a() + cb["ident"].a(), pst.a(), inc=True)
                act(lambda e, tc=tc, pst=pst: e.activation(out=cT.t[:, tc * 128:(tc + 1) * 128], in_=pst.t[:].bitcast(BF16)[:, 0:128], func=AF.Identity),
                    pst.a(), cT.a(tc))

            if stop_stage == 6:
                finish()
                return
            for qb in range(2):
                jq = 2 * ti + qb
                nkc = 2 * jq + 2
                qs = slice(qb * 256, (qb + 1) * 256)
                for h in range(8):
                    hp = h % 2
                    half = hp * 256
                    pend = None

                    OS = OSps[hp]

                    def pv(kc, PT, first, last, h=h, OS=OS):
                        pe(lambda e: e.matmul(OS.t[:, 0:256], lhsT=Vh.t[:, kc, h * 128:(h + 1) * 128], rhs=PT.t[:],
                                              start=first, stop=last, skip_group_check=True), Vh.a(kc) + PT.a(), OS.a(), inc=False)
                        pe(lambda e: e.matmul(OS.t[:, 256:512], lhsT=ones_bf.t[:], rhs=PT.t[:], start=False, stop=last, skip_group_check=True),
                           ones_bf.a() + PT.a(), OS.a(), inc=True)

                    for kc in range(nkc):
                        n = kc // 2
                        si = state["S"]
                        state["S"] = 1 - si
                        Sb = Sps[si]
                        own = (n == jq)
                        pe(lambda e, kc=kc, Sb=Sb, h=h, qs=qs: e.matmul(Sb.t[:, 0:256], lhsT=kT.t[:, h, kc * 128:(kc + 1) * 128], rhs=qT[:, h, qs],
                                                                 start=True, stop=False), kT.a(kc) + Q_ATOMS, Sb.a(), inc=False)
                        r = h * 8 + n
                        pe(lambda e, Sb=Sb, r=r, own=own, qs=qs: e.matmul(Sb.t[:, 0:256], lhsT=cb["ind"].t[:, r:r + 1].to_broadcast([128, 128]),
                                                                   rhs=cT.t[:, qs], start=False, stop=(not own)),
                           cb["ind"].a() + cT.a(2 * qb, 2 * qb + 2), Sb.a(), inc=(not own))
                        if own:
                            kcl = kc - 2 * jq
                            pe(lambda e, Sb=Sb, kcl=kcl: e.matmul(Sb.t[:, 0:256], lhsT=cb["ident"].t[:], rhs=cb["cmask"].t[:, kcl, :],
                                                                  start=False, stop=True), cb["ident"].a() + cb["cmask"].a(), Sb.a(), inc=True)
                        pi = state["PT"]
                        state["PT"] = (pi + 1) % 3
                        PT = PTs[pi]
                        didx = 2 * jq - kc + 1
                        act(lambda e, Sb=Sb, PT=PT, h=h, didx=didx: e.activation(out=PT.t[:], in_=Sb.t[:, 0:256], func=AF.Exp,
                                                                                 bias=cb["akey"].t[:, h, didx:didx + 1], scale=1.0),
                            Sb.a() + cb["akey"].a(), PT.a())
                        if pend is not None:
                            pv(*pend)
                        pend = (kc, PT, kc == 0, kc == nkc - 1)
                    pv(*pend)
                    rc, _ = next_tmp()
                    dve(lambda e, rc=rc, OS=OS: e.reciprocal(out=rc.t[:, 0:256], in_=OS.t[:, 256:512]), OS.a(), rc.a())
                    dve(lambda e, rc=rc, OS=OS, h=h, qs=qs: e.tensor_tensor(out=ybf[:, h, qs], in0=OS.t[:, 0:256], in1=rc.t[:, 0:256], op=ALU.mult),
                        OS.a() + rc.a(), Y_ATOMS)
            if stop_stage == 7:
                finish()
                return
            do_branch(2)

            if ti + 1 < n_tiles:
                load_xbf(l, ti + 1, XB[0])

            src_d = xT_d if l == 0 else scr_d
            for dc in range(16):
                rd = [] if l == 0 else [scr_atoms[ti][dc]]
                dma(lambda e, dc=dc, src_d=src_d: e.dma_start(out=R.t[:, dc, :], in_=src_d[dc * 128:(dc + 1) * 128, ti * TT:(ti + 1) * TT]),
                    rd, R.a(dc), ch_R[dc])

            for dc in range(16):
                wb = nxt()
                psb = next_mm()
                mm_f1(wb, 0, 16, mixact, MIXF.a(), psb)
                dve(lambda e, dc=dc, psb=psb: e.scalar_tensor_tensor(out=R.t[:, dc, :], in0=R.t[:, dc, :], scalar=ALPHA, in1=psb.t[:],
                                                                     op0=ALU.mult, op1=ALU.add), R.a(dc) + psb.a(), R.a(dc))
            layer_norm_R("ln1g", "ln1b", xb1)

            if stop_stage == 8:
                finish()
                return
            for fg in range(4):
                for fc in range(16):
                    wb = nxt()
                    psb = next_mm()
                    mm_f1(wb, 0, 16, x1act, xb1.a(), psb)
                    t1, _ = next_tmp()
                    dve(lambda e, psb=psb, t1=t1: e.tensor_scalar(out=T(t1), in0=psb.t[:], scalar1=0.0, scalar2=None, op0=ALU.max), psb.a(), t1.a())
                    act(lambda e, fc=fc, t1=t1: e.activation(out=MIXF.t[:, fc, :], in_=T(t1), func=AF.Square), t1.a(), MIXF.a(fc))
                for dc in range(16):
                    wb = nxt()
                    psb = next_mm()
                    mm_f1(wb, 0, 16, mixact, MIXF.a(), psb)
                    if fg == 0:
                        dve(lambda e, dc=dc, psb=psb: e.scalar_tensor_tensor(out=R.t[:, dc, :], in0=R.t[:, dc, :], scalar=ALPHA, in1=psb.t[:],
                                                                             op0=ALU.mult, op1=ALU.add), R.a(dc) + psb.a(), R.a(dc))
                    else:
                        dve(lambda e, dc=dc, psb=psb: e.tensor_tensor(out=R.t[:, dc, :], in0=R.t[:, dc, :], in1=psb.t[:], op=ALU.add),
                            R.a(dc) + psb.a(), R.a(dc))
            for dc in range(16):
                wb = nxt()
                pg = next_mm()
                mm_f1(wb, 0, 16, x1act, xb1.a(), pg)
                pa = next_mm()
                mm_f1(wb, 2048, 2, lambda kc: pTb.t[:, kc, :], pTb.a(), pa)
                sg, _ = next_tmp()
                act(lambda e, pg=pg, sg=sg: e.activation(out=T(sg), in_=pg.t[:], func=AF.Sigmoid), pg.a(), sg.a())
                dve(lambda e, pa=pa, sg=sg: e.tensor_tensor(out=T(sg), in0=pa.t[:], in1=T(sg), op=ALU.mult), pa.a() + sg.a(), sg.a())
                dve(lambda e, dc=dc, sg=sg: e.tensor_tensor(out=R.t[:, dc, :], in0=R.t[:, dc, :], in1=T(sg), op=ALU.add), R.a(dc) + sg.a(), R.a(dc))
            assert ui[0] == len(UNITS)
            layer_norm_R("ln2g", "ln2b", None)

            last = (l == n_layers - 1)
            dst_d = out_d if last else scr_d
            for dc in range(16):
                wr = [] if last else [scr_atoms[ti][dc]]
                dma(lambda e, dc=dc, dst_d=dst_d: e.dma_start(out=dst_d[dc * 128:(dc + 1) * 128, ti * TT:(ti + 1) * TT], in_=R.t[:, dc, :]),
                    R.a(dc), wr, ch_R[dc])

        for ti in range(n_tiles):
            tile_body(l, ti)

    for h in sc.sems:
        nc.gpsimd.sem_clear(h)
    nc.all_engine_barrier()
    with nc.Block() as block:
        @block.sync
        def _(e):
            sc.emit("sp", e)

        @block.tensor
        def _(e):
            sc.emit("pe", e)

        @block.scalar
        def _(e):
            sc.emit("act", e)

        @block.vector
        def _(e):
            sc.emit("dve", e)

        @block.gpsimd
        def _(e):
            sc.emit("pool", e)
    build_program.stats = {k: len(v.ops) for k, v in sc.eng.items()}
    for h in sc.sems:
        nc.gpsimd.sem_clear(h)
    nc.all_engine_barrier()
    return nc


def make_in_maps(x, p, w_in, w_sp, b_sp, vn_g, vn_b, w_pool, pool_scale, w_branch, w_out,
                 ln1_g, ln1_b, w_up, w_down, w_ple, w_ple_gate, ln2_g, ln2_b, cores=range(8)):
    f = lambda a: np.asarray(a, dtype=np.float32)
    x, p = f(x), f(p)
    ws = np.stack([build_wstream(f(w_in[l]), f(w_pool[l]), f(w_branch[l]), f(w_out[l]), f(w_up[l]), f(w_down[l]),
                                 f(w_ple[l]), f(w_ple_gate[l])) for l in range(DEPTH)])
    consts = build_consts()
    col = lambda v: np.ascontiguousarray(f(v).reshape(DEPTH, -1, 128).transpose(0, 2, 1))
    lc = {
        "ln1g": col(ln1_g), "ln1b": col(ln1_b), "ln2g": col(ln2_g), "ln2b": col(ln2_b),
        "pscale": col(pool_scale),
        "vng": np.ascontiguousarray(np.broadcast_to(f(vn_g)[:, None, :], (DEPTH, 128, MIXW))),
        "vnb": np.ascontiguousarray(np.broadcast_to(f(vn_b)[:, None, :], (DEPTH, 128, MIXW))),
        "bsp": np.ascontiguousarray(np.broadcast_to(f(b_sp)[:, None, :, :], (DEPTH, 128, 4, 128))),
        "wspT": np.ascontiguousarray(f(w_sp).transpose(0, 3, 1, 2)),
    }
    in_maps = []
    for b in cores:
        m = {"xT": np.ascontiguousarray(x[b].T), "pT": np.ascontiguousarray(p[:, b].transpose(0, 2, 1)), "wstream": ws}
        for k, v in consts.items():
            m["c_" + k] = v
        for k, v in lc.items():
            m["l_" + k] = v
        in_maps.append(m)
    return in_maps


_NC_CACHE = {}


def kernel(**inputs):
    in_maps = make_in_maps(**inputs)
    if "nc" not in _NC_CACHE:
        _NC_CACHE["nc"] = build_program()
    nc = _NC_CACHE["nc"]
    res = run_bass_kernel_spmd(nc, in_maps, core_ids=list(range(8)))
    out = np.stack([np.ascontiguousarray(r["outT"].T) for r in res.results], axis=0)
    return out.astype(np.float32)
```

```python
import numpy as np
import concourse.bass as bass
import concourse.mybir as mybir
from concourse.bass_utils import run_bass_kernel_spmd

F32 = mybir.dt.float32
BF16 = mybir.dt.bfloat16
AF = mybir.ActivationFunctionType
ALU = mybir.AluOpType
AX = mybir.AxisListType

D = 2048
S = 2048
DEPTH = 2
MIXW = 1024
DFF = 8192
PLE = 256
TT = 512
NT = S // TT
NH = 8
ALPHA = (2.0 * DEPTH) ** 0.25
LN_EPS = 1e-5
NEGM = -30000.0
USZ = 2048
PGSZ = 2304


class Atom:
    __slots__ = ("w", "r", "x")

    def __init__(self, x=False):
        self.w = None
        self.r = {}
        self.x = x


class Buf:
    def __init__(self, t, natoms=1):
        self.t = t
        self.atoms = [Atom() for _ in range(natoms)]

    def a(self, i=None, j=None):
        if i is None:
            return self.atoms
        if j is None:
            return [self.atoms[i]]
        return self.atoms[i:j]


class Chan:
    def __init__(self, sem):
        self.sem = sem
        self.count = 0


class EngState:
    def __init__(self, name, sem):
        self.name = name
        self.sem = sem
        self.count = 0
        self.seen = {}
        self.ops = []


class Sched:
    def __init__(self, nc):
        self.nc = nc
        self.sems = []
        self.eng = {}
        for n in ("pe", "act", "dve", "pool"):
            self.eng[n] = EngState(n, self.new_sem("s_" + n))
        self.eng["sp"] = EngState("sp", None)
        self.chans = []

    def new_sem(self, name):
        h = self.nc.alloc_semaphore(name)
        self.sems.append(h)
        return len(self.sems) - 1

    def chan(self, name):
        c = Chan(self.new_sem("c_" + name))
        self.chans.append(c)
        return c

    def issue(self, engname, fn, reads=(), writes=(), inc=True, chan=None):
        e = self.eng[engname]
        waits = {}

        def need(tok):
            if tok is None:
                return
            s, v = tok
            if engname == "pe" and s == e.sem:
                return
            if waits.get(s, 0) < v:
                waits[s] = v

        reads = list(reads)
        writes = list(writes)
        xr = [a for a in reads if a.x]
        if xr:
            reads = [a for a in reads if not a.x]
            writes = writes + [a for a in xr if a not in writes]
        for a in reads:
            need(a.w)
        for a in writes:
            need(a.w)
            for s, v in a.r.items():
                need((s, v))
        wl = []
        for s, v in waits.items():
            if e.seen.get(s, 0) < v:
                e.seen[s] = v
                wl.append((s, v))
        if engname == "sp":
            chan.count += 16
            tok = (chan.sem, chan.count)
            incinfo = (chan.sem, 16)
        else:
            if inc:
                e.count += 1
                tok = (e.sem, e.count)
                incinfo = (e.sem, 1)
            else:
                tok = (e.sem, e.count + 1)
                incinfo = None
        e.ops.append((wl, fn, incinfo))
        for a in reads:
            if a.r.get(tok[0], 0) < tok[1]:
                a.r[tok[0]] = tok[1]
        for a in writes:
            a.w = tok
            a.r = {}

    def emit(self, engname, engine):
        e = self.eng[engname]
        for wl, fn, incinfo in e.ops:
            for s, v in wl:
                engine.wait_ge(self.sems[s], v)
            ins = fn(engine)
            if incinfo is not None:
                ins.then_inc(self.sems[incinfo[0]], incinfo[1])
        if engname == "sp":
            for c in self.chans:
                if c.count > 0:
                    engine.wait_ge(self.sems[c.sem], c.count)


def unit_list():
    u = []
    for c in range(8):
        u.append(("gv", c))
    for c in range(8):
        u.append(("gu", c))

    def branch(n):
        for dcp in range(8):
            u.append(("gate", n, 2 * dcp))
            u.append(("gate", n, 2 * dcp + 1))
            u.append(("br", n, dcp))

    branch(0)
    for c in range(8):
        u.append(("pz", c))
    u.append(("pw",))
    branch(1)
    for c in range(8):
        u.append(("q", c))
    for c in range(8):
        u.append(("k", c))
    for c in range(8):
        u.append(("av", c))
    branch(2)
    for dc in range(16):
        u.append(("wo", dc))
    for fg in range(4):
        for fc in range(16):
            u.append(("up", fg, fc))
        for dc in range(16):
            u.append(("dn", fg, dc))
    for dc in range(16):
        u.append(("pg", dc))
    return u


UNITS = unit_list()
UOFF = []
_o = 0
for _u in UNITS:
    UOFF.append(_o)
    _o += PGSZ if _u[0] == "pg" else USZ
WTOT = _o


def _kunit(W):
    K = W.shape[0]
    return np.ascontiguousarray(W.reshape(K // 128, 128, 128).transpose(1, 0, 2)).reshape(128, K)


def build_wstream(w_in, w_pool, w_branch, w_out, w_up, w_down, w_ple, w_ple_gate):
    out = np.empty((128, WTOT), dtype=np.float32)
    win_c = lambda c: w_in[:, c * 128:(c + 1) * 128]
    for ui, u in enumerate(UNITS):
        o = UOFF[ui]
        k = u[0]
        if k == "gv":
            a = _kunit(win_c(8 + u[1]))
        elif k == "gu":
            a = _kunit(win_c(u[1]))
        elif k == "gate":
            a = _kunit(win_c(48 + u[1] * 16 + u[2]))
        elif k == "br":
            n, dcp = u[1], u[2]
            a = np.concatenate([_kunit(w_branch[n][:, (2 * dcp + j) * 128:(2 * dcp + j + 1) * 128]) for j in range(2)], axis=1)
        elif k == "pz":
            a = _kunit(win_c(16 + u[1]))
        elif k == "pw":
            a = np.ascontiguousarray(w_pool.reshape(4, 2, 128, 256).transpose(2, 0, 1, 3)).reshape(128, 2048)
        elif k == "q":
            a = _kunit(win_c(24 + u[1]))
        elif k == "k":
            a = _kunit(win_c(32 + u[1]))
        elif k == "av":
            a = _kunit(win_c(40 + u[1]))
        elif k == "wo":
            a = _kunit(w_out[:, u[1] * 128:(u[1] + 1) * 128])
        elif k == "up":
            c = u[1] * 16 + u[2]
            a = _kunit(w_up[:, c * 128:(c + 1) * 128])
        elif k == "dn":
            fg, dc = u[1], u[2]
            a = _kunit(w_down[fg * 2048:(fg + 1) * 2048, dc * 128:(dc + 1) * 128])
        elif k == "pg":
            dc = u[1]
            a = np.concatenate([_kunit(w_ple_gate[:, dc * 128:(dc + 1) * 128]),
                                _kunit(w_ple[:, dc * 128:(dc + 1) * 128])], axis=1)
        out[:, o:o + a.shape[1]] = a
    return out


def build_consts():
    c = {}
    c["ident"] = np.eye(128, dtype=np.float32)
    c["ones"] = np.ones((128, 128), dtype=np.float32)
    ind = np.zeros((128, 64), dtype=np.float32)
    for k in range(128):
        ind[k, k % 64] = 1.0
    c["ind"] = ind
    s = np.arange(128)[:, None]
    t = np.arange(128)[None, :]
    tri = np.where(s <= t, 0.0, NEGM).astype(np.float32)
    cm = np.zeros((128, 2, 256), dtype=np.float32)
    cm[:, 0, 0:128] = tri
    cm[:, 0, 128:256] = 0.0
    cm[:, 1, 0:128] = NEGM
    cm[:, 1, 128:256] = tri
    c["cmask"] = cm
    slopes = np.exp2(-8.0 * np.arange(1, 9, dtype=np.float64) / 8.0)
    ak = np.zeros((128, 8, 16), dtype=np.float64)
    for idx in range(16):
        ak[:, :, idx] = (np.arange(128)[:, None] - (idx - 1) * 128) * slopes[None, :]
    c["akey"] = ak.astype(np.float32)
    pn = np.zeros((128, 8, 8), dtype=np.float32)
    no = np.ones((128, 8, 8), dtype=np.float32)
    for qb in range(8):
        for n in range(8):
            if n >= qb:
                pn[:, qb, n] = -1e30
            if n == qb:
                no[:, qb, n] = 0.0
    c["pastneg"] = pn
    c["notown"] = no
    aq = np.zeros((128, 2, 8, 8), dtype=np.float64)
    for par in range(2):
        aq[:, par, :, :] = (-(np.arange(128)[:, None] + 128 * par) * slopes[None, :])[:, :, None]
    c["aq"] = aq.reshape(128, 2, 64).astype(np.float32)
    c["spmask"] = np.where(s <= t, 1.0, 0.0).astype(np.float32)
    ic = np.zeros((128, 4, 16), dtype=np.float32)
    for g, w in enumerate((2, 4, 8, 16)):
        ic[:, g, :] = 1.0 / np.minimum(np.arange(1, 17), w)
    c["invcnt"] = ic
    return c


CONST_SHAPES = {
    "ident": [128, 128], "ones": [128, 128], "ind": [128, 64], "cmask": [128, 2, 256],
    "akey": [128, 8, 16], "pastneg": [128, 8, 8], "notown": [128, 8, 8], "aq": [128, 2, 64],
    "spmask": [128, 128], "invcnt": [128, 4, 16],
}
BF_CONSTS = ("ident", "ones", "ind", "cmask")
LCONST_SHAPES = {
    "ln1g": [128, 16], "ln1b": [128, 16], "ln2g": [128, 16], "ln2b": [128, 16],
    "pscale": [128, 8], "vng": [128, 1024], "vnb": [128, 1024], "bsp": [128, 4, 128],
    "wspT": [128, 4, 128],
}
L_VIA_TMP = ("vng", "vnb", "wspT")


def build_program(n_layers=DEPTH, n_tiles=NT, stop_stage=99):
    nc = bass.Bass("TRN2", target_bir_lowering=False)
    sc = Sched(nc)

    def dram(name, shape, kind):
        return nc.dram_tensor(name, shape, F32, kind=kind).ap()

    xT_d = dram("xT", [D, S], "ExternalInput")
    pT_d = dram("pT", [DEPTH, PLE, S], "ExternalInput")
    ws_d = dram("wstream", [DEPTH, 128, WTOT], "ExternalInput")
    cd = {k: dram("c_" + k, v, "ExternalInput") for k, v in CONST_SHAPES.items()}
    lcd = {k: dram("l_" + k, [DEPTH] + v, "ExternalInput") for k, v in LCONST_SHAPES.items()}
    out_d = dram("outT", [D, S], "ExternalOutput")
    scr_d = dram("xscr", [D, S], "Internal") if n_layers > 1 else None
    scr_atoms = [[Atom() for _ in range(16)] for _ in range(NT)]

    def sb(name, shape, dt, natoms=1):
        return Buf(nc.alloc_sbuf_tensor(name, shape, dt), natoms)

    def ps(name, natoms=1):
        b = Buf(nc.alloc_psum_tensor(name, [128, 512], F32), natoms)
        for a in b.atoms:
            a.x = True
        return b

    kT = sb("kT", [128, NH, S], BF16, 16)
    Vh = sb("Vh", [128, 16, MIXW], BF16, 16)
    kmean = sb("kmean", [128, NH, 8], BF16, 1)
    R = sb("R", [128, 16, TT], F32, 16)
    XB = [sb("XB0", [128, 16, TT], BF16, 16), sb("XB1", [128, 16, TT], BF16, 16)]
    MIXF = sb("MIXF", [128, 16, TT], BF16, 16)
    NSTG, NWB, NTMP = 2, 2, 6
    NW = TT + 16
    stg = [sb("stg%d" % i, [128, PGSZ], F32) for i in range(NSTG)]
    wbs = [sb("wb%d" % i, [128, PGSZ], BF16) for i in range(NWB)]
    tmps = [sb("tmp%d" % i, [128, NW], F32) for i in range(NTMP)]
    halo = sb("halo", [128, 8, 16], F32, 8)
    pTb = sb("pTb", [128, 2, TT], BF16)
    cT = sb("cT", [128, TT], BF16, 4)
    PTs = [sb("PT%d" % i, [128, 256], BF16) for i in range(3)]
    small = {k: sb("sm_" + k, shp, dt) for k, shp, dt in [
        ("gm", [128, 64], F32), ("max8", [128, 8, 8], F32), ("cf", [128, 64], F32),
        ("c2", [128, 64], F32), ("chl", [128, 128], BF16), ("ksum", [128, 2], F32),
        ("bst", [128, 2, 6], F32), ("mv", [128, 2], F32), ("rstd", [128, 1], F32),
    ]}
    cb = {}
    for k, shp in CONST_SHAPES.items():
        cb[k] = sb("cb_" + k, shp, BF16 if k in BF_CONSTS else F32)
    lcb = {}
    for k, shp in LCONST_SHAPES.items():
        if k == "wspT":
            continue
        lcb[k] = sb("lcb_" + k, shp, BF16 if k in L_VIA_TMP else F32)
    wspb = sb("wspb", [128, 4, 128], BF16)
    mmps = [ps("mm%d" % i) for i in range(4)]
    Sps = [ps("S%d" % i) for i in range(2)]
    OSps = [ps("OS%d" % i) for i in range(2)]

    ch_stg = [sc.chan("stg%d" % i) for i in range(NSTG)]
    ch_tmp = [sc.chan("tmp%d" % i) for i in range(NTMP)]
    ch_R = [sc.chan("R%d" % i) for i in range(16)]

    state = {"tmp": 0, "mm": 0, "stg": 0, "wb": 0, "S": 0, "PT": 0}

    def next_tmp():
        i = state["tmp"]
        state["tmp"] = (i + 1) % NTMP
        return tmps[i], ch_tmp[i]

    def T(tb):
        return tb.t[:, 0:TT]

    def TB(tb):
        return tb.t[:].bitcast(BF16)[:, 0:TT]

    def T4(tb):
        return tb.t[:, 0:TT].rearrange("p (a b) -> p a b", a=4)

    def next_mm():
        i = state["mm"]
        state["mm"] = (i + 1) % 4
        return mmps[i]

    def pe(fn, reads, writes, inc=False):
        sc.issue("pe", fn, reads, writes, inc=inc)

    def act(fn, reads, writes):
        sc.issue("act", fn, reads, writes)

    def dve(fn, reads, writes):
        sc.issue("dve", fn, reads, writes)

    def pool(fn, reads, writes):
        sc.issue("pool", fn, reads, writes)

    def dma(fn, reads, writes, chan):
        sc.issue("sp", fn, reads, writes, chan=chan)

    def flat(ap, nd):
        return ap.rearrange("p a b -> p (a b)") if nd == 3 else ap

    def load_via_tmp(dst_ap, dst_atoms, src_ap, n, mul_ap=None, mul_atoms=()):
        for o in range(0, n, TT):
            m = min(TT, n - o)
            tb, tch = next_tmp()
            dma(lambda e, tb=tb, o=o, m=m: e.dma_start(out=tb.t[:, 0:m], in_=src_ap[:, o:o + m]), [], tb.a(), tch)
            if mul_ap is None:
                dve(lambda e, tb=tb, o=o, m=m: e.tensor_copy(out=dst_ap[:, o:o + m], in_=tb.t[:, 0:m]), tb.a(), dst_atoms)
            else:
                dve(lambda e, tb=tb, o=o, m=m: e.tensor_tensor(out=dst_ap[:, o:o + m].rearrange("p (a b) -> p a b", a=4),
                                                               in0=tb.t[:, 0:m].rearrange("p (a b) -> p a b", a=4),
                                                               in1=mul_ap, op=ALU.mult), tb.a() + list(mul_atoms), dst_atoms)

    for k, shp in CONST_SHAPES.items():
        if k in BF_CONSTS:
            load_via_tmp(flat(cb[k].t[:], len(shp)), cb[k].a(), flat(cd[k], len(shp)), int(np.prod(shp[1:])))
        else:
            ch = sc.chan("cb_" + k)
            dma(lambda e, k=k: e.dma_start(out=cb[k].t[:], in_=cd[k]), [], cb[k].a(), ch)
    dve(lambda e: e.memset(kmean.t[:], 0.0), [], kmean.a())
    lchan = {k: sc.chan("lcb_" + k) for k in LCONST_SHAPES if k not in L_VIA_TMP}

    def load_unit(l, ui):
        size = PGSZ if UNITS[ui][0] == "pg" else USZ
        off = UOFF[ui]
        si = state["stg"]
        state["stg"] = (si + 1) % NSTG
        wi = state["wb"]
        state["wb"] = (wi + 1) % NWB
        sg, wb = stg[si], wbs[wi]
        dma(lambda e: e.dma_start(out=sg.t[:, 0:size], in_=ws_d[l, :, off:off + size]), [], sg.a(), ch_stg[si])
        act(lambda e: e.activation(out=wb.t[:, 0:size], in_=sg.t[:, 0:size], func=AF.Identity), sg.a(), wb.a())
        return wb

    def mm_f1(wb, woff, nk, act_fn, act_atoms, psb):
        for kc in range(nk):
            pe(lambda e, kc=kc: e.matmul(psb.t[:], lhsT=wb.t[:, woff + kc * 128: woff + (kc + 1) * 128],
                                         rhs=act_fn(kc), start=(kc == 0), stop=(kc == nk - 1)),
               wb.a() + act_atoms, psb.a(), inc=(kc == nk - 1))

    def mm_f2(wb, xb, psb):
        for tc in range(4):
            for kc in range(16):
                pe(lambda e, tc=tc, kc=kc: e.matmul(psb.t[:, tc * 128:(tc + 1) * 128],
                                                    lhsT=xb.t[:, kc, tc * 128:(tc + 1) * 128],
                                                    rhs=wb.t[:, kc * 128:(kc + 1) * 128],
                                                    start=(kc == 0), stop=(kc == 15)),
                   wb.a() + xb.a(), psb.a(), inc=(tc == 3 and kc == 15))

    GC = 1.5957691216057308

    def gelu_from_psum(psb, out_ap_fn, out_atoms, view3=False):
        t1, _ = next_tmp()
        t2, _ = next_tmp()
        act(lambda e: e.activation(out=T(t1), in_=psb.t[:], func=AF.Identity), psb.a(), t1.a())
        dve(lambda e: e.tensor_tensor(out=T(t2), in0=T(t1), in1=T(t1), op=ALU.mult), t1.a(), t2.a())
        dve(lambda e: e.tensor_scalar(out=T(t2), in0=T(t2), scalar1=0.044715, scalar2=1.0, op0=ALU.mult, op1=ALU.add),
            t2.a(), t2.a())
        dve(lambda e: e.tensor_tensor(out=T(t2), in0=T(t2), in1=T(t1), op=ALU.mult), t1.a() + t2.a(), t2.a())
        act(lambda e: e.activation(out=T(t2), in_=T(t2), func=AF.Sigmoid, scale=GC), t2.a(), t2.a())
        if view3:
            dve(lambda e: e.tensor_tensor(out=out_ap_fn(), in0=T4(t2), in1=T4(t1), op=ALU.mult), t1.a() + t2.a(), out_atoms)
        else:
            dve(lambda e: e.tensor_tensor(out=out_ap_fn(), in0=T(t2), in1=T(t1), op=ALU.mult), t1.a() + t2.a(), out_atoms)

    ones_bf = cb["ones"]

    def mixf_f32(i):
        return MIXF.t[:, 2 * i:2 * i + 2, :].rearrange("p a b -> p (a b)").bitcast(F32)

    def layer_norm_R(gk, bk, xb_out):
        ps_sum = next_mm()
        ps_sq = next_mm()
        for dc in range(16):
            t1, _ = next_tmp()
            act(lambda e, dc=dc, t1=t1: e.activation(out=TB(t1), in_=R.t[:, dc, :], func=AF.Identity), R.a(dc), t1.a())
            t2, _ = next_tmp()
            act(lambda e, dc=dc, t2=t2: e.activation(out=TB(t2), in_=R.t[:, dc, :], func=AF.Square), R.a(dc), t2.a())
            pe(lambda e, dc=dc, t1=t1: e.matmul(ps_sum.t[:], lhsT=ones_bf.t[:], rhs=TB(t1), start=(dc == 0), stop=(dc == 15)),
               t1.a() + ones_bf.a(), ps_sum.a(), inc=True)
            pe(lambda e, dc=dc, t2=t2: e.matmul(ps_sq.t[:], lhsT=ones_bf.t[:], rhs=TB(t2), start=(dc == 0), stop=(dc == 15)),
               t2.a() + ones_bf.a(), ps_sq.a(), inc=True)
        M, A, B = mixf_f32(0), mixf_f32(1), mixf_f32(2)
        Ma, Aa, Ba = MIXF.a(0, 2), MIXF.a(2, 4), MIXF.a(4, 6)
        dve(lambda e: e.tensor_scalar(out=M, in0=ps_sum.t[:], scalar1=1.0 / D, scalar2=None, op0=ALU.mult), ps_sum.a(), Ma)
        dve(lambda e: e.tensor_tensor(out=B, in0=M, in1=M, op=ALU.mult), Ma, Ba)
        dve(lambda e: e.scalar_tensor_tensor(out=A, in0=ps_sq.t[:], scalar=1.0 / D, in1=B, op0=ALU.mult, op1=ALU.subtract),
            ps_sq.a() + Ba, Aa)
        dve(lambda e: e.tensor_scalar(out=A, in0=A, scalar1=LN_EPS, scalar2=None, op0=ALU.add), Aa, Aa)
        act(lambda e: e.activation(out=A, in_=A, func=AF.Sqrt), Aa, Aa)
        dve(lambda e: e.reciprocal(out=A, in_=A), Aa, Aa)
        dve(lambda e: e.scalar_tensor_tensor(out=B, in0=M, scalar=-1.0, in1=A, op0=ALU.mult, op1=ALU.mult), Ma + Aa, Ba)
        for dc in range(16):
            t1, _ = next_tmp()
            dve(lambda e, dc=dc, t1=t1: e.tensor_tensor(out=T(t1), in0=R.t[:, dc, :], in1=A, op=ALU.mult), R.a(dc) + Aa, t1.a())
            dve(lambda e, t1=t1: e.tensor_tensor(out=T(t1), in0=T(t1), in1=B, op=ALU.add), t1.a() + Ba, t1.a())
            dve(lambda e, dc=dc, t1=t1: e.tensor_scalar(out=R.t[:, dc, :], in0=T(t1), scalar1=lcb[gk].t[:, dc:dc + 1],
                                                        scalar2=lcb[bk].t[:, dc:dc + 1], op0=ALU.mult, op1=ALU.add),
                t1.a() + lcb[gk].a() + lcb[bk].a(), R.a(dc))
            if xb_out is not None:
                act(lambda e, dc=dc: e.activation(out=xb_out.t[:, dc, :], in_=R.t[:, dc, :], func=AF.Identity), R.a(dc), xb_out.a(dc))

    def load_xbf(l, ti, xb):
        src = xT_d if l == 0 else scr_d
        for dc in range(16):
            tb, tch = next_tmp()
            rd = [] if l == 0 else [scr_atoms[ti][dc]]
            dma(lambda e, dc=dc, tb=tb: e.dma_start(out=T(tb), in_=src[dc * 128:(dc + 1) * 128, ti * TT:(ti + 1) * TT]),
                rd, tb.a(), tch)
            dve(lambda e, dc=dc, tb=tb: e.tensor_copy(out=xb.t[:, dc, :], in_=T(tb)), tb.a(), xb.a(dc))

    def R_bf(c0, n):
        return R.t[:, c0:c0 + n, :].rearrange("p a b -> p (a b)").bitcast(BF16)

    unit_seq = [(l_, u_) for l_ in range(n_layers) for _t in range(n_tiles) for u_ in range(len(UNITS))]
    upos = [0]
    pending = []

    def nxt_global():
        if not pending:
            pending.append(load_unit(*unit_seq[upos[0]]))
        w = pending.pop(0)
        upos[0] += 1
        if upos[0] < len(unit_seq) and stop_stage == 99:
            pending.append(load_unit(*unit_seq[upos[0]]))
        return w

    for l in range(n_layers):
        for k in LCONST_SHAPES:
            if k in ("vng", "vnb"):
                load_via_tmp(lcb[k].t[:], lcb[k].a(), lcd[k][l], 1024)
            elif k == "wspT":
                load_via_tmp(wspb.t[:].rearrange("p a b -> p (a b)"), wspb.a(), lcd[k][l].rearrange("p a b -> p (a b)"), 512,
                             mul_ap=cb["spmask"].t[:].unsqueeze(1).to_broadcast([128, 4, 128]), mul_atoms=cb["spmask"].a())
            else:
                dma(lambda e, k=k, l=l: e.dma_start(out=lcb[k].t[:], in_=lcd[k][l]), [], lcb[k].a(), lchan[k])
        dve(lambda e: e.memset(halo.t[:], 0.0), [], halo.a())
        load_xbf(l, 0, XB[0])

        def tile_body(l, ti):
            xb = XB[0]
            xb1 = XB[1]
            ui = [0]

            def finish():
                for dc in range(16):
                    dma(lambda e, dc=dc: e.dma_start(out=out_d[dc * 128:(dc + 1) * 128, ti * TT:(ti + 1) * TT], in_=R.t[:, dc, :]),
                        R.a(dc), [], ch_R[dc])

            def nxt():
                assert unit_seq[upos[0]] == (l, ui[0])
                ui[0] += 1
                return nxt_global()

            xact = lambda kc: xb.t[:, kc, :]
            x1act = lambda kc: xb1.t[:, kc, :]
            mixact = lambda kc: MIXF.t[:, kc, :]

            for kc in range(2):
                tb, tch = next_tmp()
                dma(lambda e, kc=kc, tb=tb: e.dma_start(out=T(tb), in_=pT_d[l, kc * 128:(kc + 1) * 128, ti * TT:(ti + 1) * TT]),
                    [], tb.a(), tch)
                dve(lambda e, kc=kc, tb=tb: e.tensor_copy(out=pTb.t[:, kc, :], in_=T(tb)), tb.a(), pTb.a())

            vtok = R.t[:, 0:8, :].rearrange("p a b -> p (a b)").rearrange("p (t c) -> p t c", t=4)
            vbf = R_bf(8, 4).rearrange("p (t c) -> p t c", t=4)
            for c in range(8):
                wb = nxt()
                psb = next_mm()
                mm_f2(wb, xb, psb)
                gelu_from_psum(psb, lambda c=c: vtok[:, :, c * 128:(c + 1) * 128], R.a(0, 8), view3=True)
            bst, mv, rstd = small["bst"], small["mv"], small["rstd"]
            for tc in range(4):
                for hh in range(2):
                    dve(lambda e, tc=tc, hh=hh: e.bn_stats(out=bst.t[:, hh, :], in_=vtok[:, tc, hh * 512:(hh + 1) * 512]), R.a(0, 8), bst.a())
                dve(lambda e: e.bn_aggr(out=mv.t[:], in_=bst.t[:].rearrange("p a b -> p (a b)")), bst.a(), mv.a())
                dve(lambda e: e.tensor_scalar(out=rstd.t[:], in0=mv.t[:, 1:2], scalar1=LN_EPS, scalar2=None, op0=ALU.add), mv.a(), rstd.a())
                act(lambda e: e.activation(out=rstd.t[:], in_=rstd.t[:], func=AF.Sqrt), rstd.a(), rstd.a())
                dve(lambda e: e.reciprocal(out=rstd.t[:], in_=rstd.t[:]), rstd.a(), rstd.a())
                dve(lambda e, tc=tc: e.tensor_scalar(out=vtok[:, tc, :], in0=vtok[:, tc, :], scalar1=mv.t[:, 0:1], scalar2=rstd.t[:, 0:1],
                                                     op0=ALU.subtract, op1=ALU.mult), R.a(0, 8) + mv.a() + rstd.a(), R.a(0, 8))
                dve(lambda e, tc=tc: e.tensor_tensor(out=vtok[:, tc, :], in0=vtok[:, tc, :], in1=lcb["vng"].t[:], op=ALU.mult),
                    R.a(0, 8) + lcb["vng"].a(), R.a(0, 8))
                dve(lambda e, tc=tc: e.tensor_tensor(out=vbf[:, tc, :], in0=vtok[:, tc, :], in1=lcb["vnb"].t[:], op=ALU.add),
                    R.a(0, 8) + lcb["vnb"].a(), R.a(8, 12))

            if stop_stage == 1:
                finish()
                return
            ybf = R_bf(12, 4).rearrange("p (c t) -> p c t", c=8)
            Y_ATOMS = R.a(12, 16)
            yact = lambda kc: ybf[:, kc, :]

            for c in range(8):
                wb = nxt()
                psb = next_mm()
                mm_f1(wb, 0, 16, xact, xb.a(), psb)
                tu, _ = next_tmp()
                gelu_from_psum(psb, lambda tu=tu: T(tu), tu.a())
                ps2 = next_mm()
                g = c // 2
                for tc in range(4):
                    pe(lambda e, tc=tc, c=c, g=g, ps2=ps2: e.matmul(ps2.t[:, tc * 128:(tc + 1) * 128], lhsT=vbf[:, tc, c * 128:(c + 1) * 128],
                                                                    rhs=wspb.t[:, g, :], start=True, stop=True),
                       R.a(8, 12) + wspb.a(), ps2.a(), inc=(tc == 3))
                t3, _ = next_tmp()
                dve(lambda e, g=g, ps2=ps2, t3=t3: e.tensor_tensor(out=T4(t3), in0=ps2.t[:].rearrange("p (a b) -> p a b", a=4),
                                                                   in1=lcb["bsp"].t[:, g, :].unsqueeze(1).to_broadcast([128, 4, 128]), op=ALU.add),
                    ps2.a() + lcb["bsp"].a(), t3.a())
                dve(lambda e, c=c, t3=t3, tu=tu: e.tensor_tensor(out=ybf[:, c, :], in0=T(t3), in1=T(tu), op=ALU.mult),
                    t3.a() + tu.a(), Y_ATOMS)

            if stop_stage == 2:
                finish()
                return
            def do_branch(n):
                for dcp in range(8):
                    wg0 = nxt()
                    pg0 = next_mm()
                    mm_f1(wg0, 0, 16, xact, xb.a(), pg0)
                    sg0, _ = next_tmp()
                    act(lambda e, pg0=pg0, sg0=sg0: e.activation(out=T(sg0), in_=pg0.t[:], func=AF.Sigmoid), pg0.a(), sg0.a())
                    wg1 = nxt()
                    pg1 = next_mm()
                    mm_f1(wg1, 0, 16, xact, xb.a(), pg1)
                    sg1, _ = next_tmp()
                    act(lambda e, pg1=pg1, sg1=sg1: e.activation(out=T(sg1), in_=pg1.t[:], func=AF.Sigmoid), pg1.a(), sg1.a())
                    wbr = nxt()
                    for j, sg in ((0, sg0), (1, sg1)):
                        dc = 2 * dcp + j
                        pb = next_mm()
                        mm_f1(wbr, j * 1024, 8, yact, Y_ATOMS, pb)
                        if n == 0:
                            dve(lambda e, dc=dc, pb=pb, sg=sg: e.tensor_tensor(out=MIXF.t[:, dc, :], in0=pb.t[:], in1=T(sg), op=ALU.mult),
                                pb.a() + sg.a(), MIXF.a(dc))
                        else:
                            dve(lambda e, pb=pb, sg=sg: e.tensor_tensor(out=T(sg), in0=pb.t[:], in1=T(sg), op=ALU.mult),
                                pb.a() + sg.a(), sg.a())
                            dve(lambda e, dc=dc, sg=sg: e.tensor_tensor(out=MIXF.t[:, dc, :], in0=T(sg), in1=MIXF.t[:, dc, :], op=ALU.add),
                                sg.a() + MIXF.a(dc), MIXF.a(dc))

            do_branch(0)

            if stop_stage == 3:
                finish()
                return
            pooled = R_bf(0, 4).rearrange("p (c t) -> p c t", c=8)
            P_ATOMS = R.a(0, 4)
            for c in range(8):
                wb = nxt()
                psb = next_mm()
                mm_f1(wb, 0, 16, xact, xb.a(), psb)
                zc, _ = next_tmp()
                g = c // 2
                w = 2 << g
                act(lambda e, zc=zc, psb=psb: e.activation(out=zc.t[:, 16:NW], in_=psb.t[:], func=AF.Identity), psb.a(), zc.a())
                dve(lambda e, zc=zc, c=c: e.tensor_copy(out=zc.t[:, 0:16], in_=halo.t[:, c, :]), halo.a(c) + zc.a(), zc.a())
                src = zc
                sh = 1
                while sh < w:
                    dst, _ = next_tmp()
                    dve(lambda e, src=src, dst=dst, sh=sh: e.tensor_tensor(out=dst.t[:, sh:NW], in0=src.t[:, sh:NW], in1=src.t[:, 0:NW - sh], op=ALU.add),
                        src.a(), dst.a())
                    src = dst
                    sh *= 2
                dve(lambda e, src=src, zc=zc, c=c, w=w: e.scalar_tensor_tensor(out=pooled[:, c, :], in0=src.t[:, 16:NW], scalar=1.0 / w,
                                                                               in1=zc.t[:, 16:NW], op0=ALU.mult, op1=ALU.subtract),
                    src.a() + zc.a(), P_ATOMS)
                if ti == 0:
                    t1, _ = next_tmp()
                    dve(lambda e, src=src, g=g, t1=t1: e.tensor_tensor(out=t1.t[:, 0:16], in0=src.t[:, 16:32], in1=cb["invcnt"].t[:, g, :], op=ALU.mult),
                        src.a() + cb["invcnt"].a(), t1.a())
                    dve(lambda e, zc=zc, c=c, t1=t1: e.tensor_tensor(out=pooled[:, c, 0:16], in0=t1.t[:, 0:16], in1=zc.t[:, 16:32], op=ALU.subtract),
                        t1.a() + zc.a(), P_ATOMS)
                dve(lambda e, zc=zc, c=c: e.tensor_copy(out=halo.t[:, c, :], in_=zc.t[:, TT:NW]), zc.a(), halo.a(c))
            wb = nxt()
            for g in range(4):
                for hf in range(2):
                    pb = next_mm()
                    for kc in range(2):
                        o = g * 512 + kc * 256 + hf * 128
                        pe(lambda e, o=o, g=g, kc=kc, pb=pb, wb=wb: e.matmul(pb.t[:], lhsT=wb.t[:, o:o + 128], rhs=pooled[:, 2 * g + kc, :],
                                                                             start=(kc == 0), stop=(kc == 1)),
                           wb.a() + P_ATOMS, pb.a(), inc=(kc == 1))
                    cc = 2 * g + hf
                    dve(lambda e, cc=cc, pb=pb: e.tensor_scalar(out=ybf[:, cc, :], in0=pb.t[:], scalar1=lcb["pscale"].t[:, cc:cc + 1], scalar2=None, op0=ALU.mult),
                        pb.a() + lcb["pscale"].a(), Y_ATOMS)
            if stop_stage == 4:
                finish()
                return
            do_branch(1)

            qT = R_bf(0, 4).rearrange("p (c t) -> p c t", c=8)
            Q_ATOMS = R.a(0, 4)
            for h in range(8):
                wb = nxt()
                psb = next_mm()
                mm_f1(wb, 0, 16, xact, xb.a(), psb)
                act(lambda e, h=h, psb=psb: e.activation(out=qT[:, h, :], in_=psb.t[:], func=AF.Identity, scale=float(128 ** -0.5)), psb.a(), Q_ATOMS)
            ks = small["ksum"]
            for h in range(8):
                wb = nxt()
                psb = next_mm()
                mm_f1(wb, 0, 16, xact, xb.a(), psb)
                act(lambda e, h=h, psb=psb: e.activation(out=kT.t[:, h, ti * TT:(ti + 1) * TT], in_=psb.t[:], func=AF.Identity),
                    psb.a(), kT.a(4 * ti, 4 * ti + 4))
                dve(lambda e, psb=psb: e.tensor_reduce(out=ks.t[:], in_=psb.t[:].rearrange("p (a b) -> p a b", a=2), axis=AX.X, op=ALU.add), psb.a(), ks.a())
                dve(lambda e, h=h: e.tensor_scalar(out=kmean.t[:, h, 2 * ti:2 * ti + 2], in0=ks.t[:], scalar1=1.0 / 256, scalar2=None, op0=ALU.mult),
                    ks.a(), kmean.a())
            for h in range(8):
                wb = nxt()
                psb = next_mm()
                mm_f2(wb, xb, psb)
                act(lambda e, h=h, psb=psb: e.activation(out=Vh.t[:, 4 * ti:4 * ti + 4, h * 128:(h + 1) * 128],
                                                         in_=psb.t[:].rearrange("p (a b) -> p a b", a=4), func=AF.Identity),
                    psb.a(), Vh.a(4 * ti, 4 * ti + 4))

            if stop_stage == 5:
                finish()
                return
            gm, max8, cf, c2, chl = small["gm"], small["max8"], small["cf"], small["c2"], small["chl"]
            v88 = lambda ap: ap.rearrange("p (a b) -> p a b", a=8)
            for tc in range(4):
                qblk = (ti * TT + tc * 128) // 256
                psg = next_mm()
                for h in range(8):
                    pe(lambda e, h=h, tc=tc, psg=psg: e.matmul(psg.t[:, h * 8:(h + 1) * 8], lhsT=qT[:, h, tc * 128:(tc + 1) * 128], rhs=kmean.t[:, h, :],
                                                               start=True, stop=True), Q_ATOMS + kmean.a(), psg.a(), inc=(h == 7))
                dve(lambda e, psg=psg, qblk=qblk: e.tensor_tensor(out=v88(gm.t[:]), in0=v88(psg.t[:, 0:64]),
                                                                  in1=cb["pastneg"].t[:, qblk, :].unsqueeze(1).to_broadcast([128, 8, 8]), op=ALU.add),
                    psg.a() + cb["pastneg"].a(), gm.a())
                for h in range(8):
                    dve(lambda e, h=h: e.max(out=max8.t[:, h, :], in_=gm.t[:, h * 8:(h + 1) * 8]), gm.a(), max8.a())
                for h in range(8):
                    dve(lambda e, h=h: e.tensor_scalar(out=cf.t[:, h * 8:(h + 1) * 8], in0=gm.t[:, h * 8:(h + 1) * 8], scalar1=max8.t[:, h, 2:3],
                                                       scalar2=NEGM, op0=ALU.is_lt, op1=ALU.mult), gm.a() + max8.a(), cf.a())
                dve(lambda e, qblk=qblk: e.tensor_tensor(out=v88(cf.t[:]), in0=v88(cf.t[:]),
                                                         in1=cb["notown"].t[:, qblk, :].unsqueeze(1).to_broadcast([128, 8, 8]), op=ALU.mult),
                    cf.a() + cb["notown"].a(), cf.a())
                dve(lambda e, tc=tc: e.tensor_tensor(out=c2.t[:], in0=cf.t[:], in1=cb["aq"].t[:, tc % 2, :], op=ALU.add), cf.a() + cb["aq"].a(), c2.a())
                dve(lambda e: e.tensor_copy(out=chl.t[:, 0:64], in_=c2.t[:]), c2.a(), chl.a())
                dve(lambda e: e.tensor_tensor(out=chl.t[:, 64:128], in0=c2.t[:], in1=chl.t[:, 0:64], op=ALU.subtract), c2.a() + chl.a(), chl.a())
                pst = next_mm()
                pe(lambda e, pst=pst: e.transpose(out=pst.t[:].bitcast(BF16)[:, 0:128], in_=chl.t[:], identity=cb["ident"].t[:]),
                   chl.a() + cb["ident"].a(), pst.a(), inc=True)
                act(lambda e, tc=tc, pst=pst: e.activation(out=cT.t[:, tc * 128:(tc + 1) * 128], in_=pst.t[:].bitcast(BF16)[:, 0:128], func=AF.Identity),
                    pst.a(), cT.a(tc))

            if stop_stage == 6:
                finish()
                return
            for qb in range(2):
                jq = 2 * ti + qb
                nkc = 2 * jq + 2
                qs = slice(qb * 256, (qb + 1) * 256)
                for h in range(8):
                    hp = h % 2
                    half = hp * 256
                    pend = None

                    OS = OSps[hp]

                    def pv(kc, PT, first, last, h=h, OS=OS):
                        pe(lambda e: e.matmul(OS.t[:, 0:256], lhsT=Vh.t[:, kc, h * 128:(h + 1) * 128], rhs=PT.t[:],
                                              start=first, stop=last, skip_group_check=True), Vh.a(kc) + PT.a(), OS.a(), inc=False)
                        pe(lambda e: e.matmul(OS.t[:, 256:512], lhsT=ones_bf.t[:], rhs=PT.t[:], start=False, stop=last, skip_group_check=True),
                           ones_bf.a() + PT.a(), OS.a(), inc=True)

                    for kc in range(nkc):
                        n = kc // 2
                        si = state["S"]
                        state["S"] = 1 - si
                        Sb = Sps[si]
                        own = (n == jq)
                        pe(lambda e, kc=kc, Sb=Sb, h=h, qs=qs: e.matmul(Sb.t[:, 0:256], lhsT=kT.t[:, h, kc * 128:(kc + 1) * 128], rhs=qT[:, h, qs],
                                                                 start=True, stop=False), kT.a(kc) + Q_ATOMS, Sb.a(), inc=False)
                        r = h * 8 + n
                        pe(lambda e, Sb=Sb, r=r, own=own, qs=qs: e.matmul(Sb.t[:, 0:256], lhsT=cb["ind"].t[:, r:r + 1].to_broadcast([128, 128]),
                                                                   rhs=cT.t[:, qs], start=False, stop=(not own)),
                           cb["ind"].a() + cT.a(2 * qb, 2 * qb + 2), Sb.a(), inc=(not own))
                        if own:
                            kcl = kc - 2 * jq
                            pe(lambda e, Sb=Sb, kcl=kcl: e.matmul(Sb.t[:, 0:256], lhsT=cb["ident"].t[:], rhs=cb["cmask"].t[:, kcl, :],
                                                                  start=False, stop=True), cb["ident"].a() + cb["cmask"].a(), Sb.a(), inc=True)
                        pi = state["PT"]
                        state["PT"] = (pi + 1) % 3
                        PT = PTs[pi]
                        didx = 2 * jq - kc + 1
                        act(lambda e, Sb=Sb, PT=PT, h=h, didx=didx: e.activation(out=PT.t[:], in_=Sb.t[:, 0:256], func=AF.Exp,
                                                                                 bias=cb["akey"].t[:, h, didx:didx + 1], scale=1.0),
                            Sb.a() + cb["akey"].a(), PT.a())
                        if pend is not None:
                            pv(*pend)
                        pend = (kc, PT, kc == 0, kc == nkc - 1)
                    pv(*pend)
                    rc, _ = next_tmp()
                    dve(lambda e, rc=rc, OS=OS: e.reciprocal(out=rc.t[:, 0:256], in_=OS.t[:, 256:512]), OS.a(), rc.a())
                    dve(lambda e, rc=rc, OS=OS, h=h, qs=qs: e.tensor_tensor(out=ybf[:, h, qs], in0=OS.t[:, 0:256], in1=rc.t[:, 0:256], op=ALU.mult),
                        OS.a() + rc.a(), Y_ATOMS)
            if stop_stage == 7:
                finish()
                return
            do_branch(2)

            if ti + 1 < n_tiles:
                load_xbf(l, ti + 1, XB[0])

            src_d = xT_d if l == 0 else scr_d
            for dc in range(16):
                rd = [] if l == 0 else [scr_atoms[ti][dc]]
                dma(lambda e, dc=dc, src_d=src_d: e.dma_start(out=R.t[:, dc, :], in_=src_d[dc * 128:(dc + 1) * 128, ti * TT:(ti + 1) * TT]),
                    rd, R.a(dc), ch_R[dc])

            for dc in range(16):
                wb = nxt()
                psb = next_mm()
                mm_f1(wb, 0, 16, mixact, MIXF.a(), psb)
                dve(lambda e, dc=dc, psb=psb: e.scalar_tensor_tensor(out=R.t[:, dc, :], in0=R.t[:, dc, :], scalar=ALPHA, in1=psb.t[:],
                                                                     op0=ALU.mult, op1=ALU.add), R.a(dc) + psb.a(), R.a(dc))
            layer_norm_R("ln1g", "ln1b", xb1)

            if stop_stage == 8:
                finish()
                return
            for fg in range(4):
                for fc in range(16):
                    wb = nxt()
                    psb = next_mm()
                    mm_f1(wb, 0, 16, x1act, xb1.a(), psb)
                    t1, _ = next_tmp()
                    dve(lambda e, psb=psb, t1=t1: e.tensor_scalar(out=T(t1), in0=psb.t[:], scalar1=0.0, scalar2=None, op0=ALU.max), psb.a(), t1.a())
                    act(lambda e, fc=fc, t1=t1: e.activation(out=MIXF.t[:, fc, :], in_=T(t1), func=AF.Square), t1.a(), MIXF.a(fc))
                for dc in range(16):
                    wb = nxt()
                    psb = next_mm()
                    mm_f1(wb, 0, 16, mixact, MIXF.a(), psb)
                    if fg == 0:
                        dve(lambda e, dc=dc, psb=psb: e.scalar_tensor_tensor(out=R.t[:, dc, :], in0=R.t[:, dc, :], scalar=ALPHA, in1=psb.t[:],
                                                                             op0=ALU.mult, op1=ALU.add), R.a(dc) + psb.a(), R.a(dc))
                    else:
                        dve(lambda e, dc=dc, psb=psb: e.tensor_tensor(out=R.t[:, dc, :], in0=R.t[:, dc, :], in1=psb.t[:], op=ALU.add),
                            R.a(dc) + psb.a(), R.a(dc))
            for dc in range(16):
                wb = nxt()
                pg = next_mm()
                mm_f1(wb, 0, 16, x1act, xb1.a(), pg)
                pa = next_mm()
                mm_f1(wb, 2048, 2, lambda kc: pTb.t[:, kc, :], pTb.a(), pa)
                sg, _ = next_tmp()
                act(lambda e, pg=pg, sg=sg: e.activation(out=T(sg), in_=pg.t[:], func=AF.Sigmoid), pg.a(), sg.a())
                dve(lambda e, pa=pa, sg=sg: e.tensor_tensor(out=T(sg), in0=pa.t[:], in1=T(sg), op=ALU.mult), pa.a() + sg.a(), sg.a())
                dve(lambda e, dc=dc, sg=sg: e.tensor_tensor(out=R.t[:, dc, :], in0=R.t[:, dc, :], in1=T(sg), op=ALU.add), R.a(dc) + sg.a(), R.a(dc))
            assert ui[0] == len(UNITS)
            layer_norm_R("ln2g", "ln2b", None)

            last = (l == n_layers - 1)
            dst_d = out_d if last else scr_d
            for dc in range(16):
                wr = [] if last else [scr_atoms[ti][dc]]
                dma(lambda e, dc=dc, dst_d=dst_d: e.dma_start(out=dst_d[dc * 128:(dc + 1) * 128, ti * TT:(ti + 1) * TT], in_=R.t[:, dc, :]),
                    R.a(dc), wr, ch_R[dc])

        for ti in range(n_tiles):
            tile_body(l, ti)

    for h in sc.sems:
        nc.gpsimd.sem_clear(h)
    nc.all_engine_barrier()
    with nc.Block() as block:
        @block.sync
        def _(e):
            sc.emit("sp", e)

        @block.tensor
        def _(e):
            sc.emit("pe", e)

        @block.scalar
        def _(e):
            sc.emit("act", e)

        @block.vector
        def _(e):
            sc.emit("dve", e)

        @block.gpsimd
        def _(e):
            sc.emit("pool", e)
    build_program.stats = {k: len(v.ops) for k, v in sc.eng.items()}
    build_program.sbuf_left = nc.sbuf_bytes_remaining
    for h in sc.sems:
        nc.gpsimd.sem_clear(h)
    nc.all_engine_barrier()
    return nc


def make_in_maps(x, p, w_in, w_sp, b_sp, vn_g, vn_b, w_pool, pool_scale, w_branch, w_out,
                 ln1_g, ln1_b, w_up, w_down, w_ple, w_ple_gate, ln2_g, ln2_b, cores=range(8)):
    f = lambda a: np.asarray(a, dtype=np.float32)
    x, p = f(x), f(p)
    ws = np.stack([build_wstream(f(w_in[l]), f(w_pool[l]), f(w_branch[l]), f(w_out[l]), f(w_up[l]), f(w_down[l]),
                                 f(w_ple[l]), f(w_ple_gate[l])) for l in range(DEPTH)])
    consts = build_consts()
    col = lambda v: np.ascontiguousarray(f(v).reshape(DEPTH, -1, 128).transpose(0, 2, 1))
    lc = {
        "ln1g": col(ln1_g), "ln1b": col(ln1_b), "ln2g": col(ln2_g), "ln2b": col(ln2_b),
        "pscale": col(pool_scale),
        "vng": np.ascontiguousarray(np.broadcast_to(f(vn_g)[:, None, :], (DEPTH, 128, MIXW))),
        "vnb": np.ascontiguousarray(np.broadcast_to(f(vn_b)[:, None, :], (DEPTH, 128, MIXW))),
        "bsp": np.ascontiguousarray(np.broadcast_to(f(b_sp)[:, None, :, :], (DEPTH, 128, 4, 128))),
        "wspT": np.ascontiguousarray(f(w_sp).transpose(0, 3, 1, 2)),
    }
    in_maps = []
    for b in cores:
        m = {"xT": np.ascontiguousarray(x[b].T), "pT": np.ascontiguousarray(p[:, b].transpose(0, 2, 1)), "wstream": ws}
        for k, v in consts.items():
            m["c_" + k] = v
        for k, v in lc.items():
            m["l_" + k] = v
        in_maps.append(m)
    return in_maps


_NC_CACHE = {}


def kernel(**inputs):
    in_maps = make_in_maps(**inputs)
    if "nc" not in _NC_CACHE:
        _NC_CACHE["nc"] = build_program()
    nc = _NC_CACHE["nc"]
    res = run_bass_kernel_spmd(nc, in_maps, core_ids=list(range(8)))
    out = np.stack([np.ascontiguousarray(r["outT"].T) for r in res.results], axis=0)
    return out.astype(np.float32)
```

```python
import numpy as np
import concourse.bass as bass
import concourse.mybir as mybir
from concourse.bass_utils import run_bass_kernel_spmd

F32 = mybir.dt.float32
BF16 = mybir.dt.bfloat16
AF = mybir.ActivationFunctionType
ALU = mybir.AluOpType
AX = mybir.AxisListType

D = 2048
S = 2048
DEPTH = 2
MIXW = 1024
DFF = 8192
PLE = 256
TT = 512
NT = S // TT
NH = 8
ALPHA = (2.0 * DEPTH) ** 0.25
LN_EPS = 1e-5
NEGM = -30000.0
USZ = 2048
PGSZ = 2304


class Atom:
    __slots__ = ("w", "r", "x")

    def __init__(self, x=False):
        self.w = None
        self.r = {}
        self.x = x


class Buf:
    def __init__(self, t, natoms=1):
        self.t = t
        self.atoms = [Atom() for _ in range(natoms)]

    def a(self, i=None, j=None):
        if i is None:
            return self.atoms
        if j is None:
            return [self.atoms[i]]
        return self.atoms[i:j]


class Chan:
    def __init__(self, sem):
        self.sem = sem
        self.count = 0


class EngState:
    def __init__(self, name, sem):
        self.name = name
        self.sem = sem
        self.count = 0
        self.seen = {}
        self.ops = []


class Sched:
    def __init__(self, nc):
        self.nc = nc
        self.sems = []
        self.eng = {}
        for n in ("pe", "act", "dve", "pool"):
            self.eng[n] = EngState(n, self.new_sem("s_" + n))
        self.eng["sp"] = EngState("sp", None)
        self.chans = []

    def new_sem(self, name):
        h = self.nc.alloc_semaphore(name)
        self.sems.append(h)
        return len(self.sems) - 1

    def chan(self, name):
        c = Chan(self.new_sem("c_" + name))
        self.chans.append(c)
        return c

    def issue(self, engname, fn, reads=(), writes=(), inc=True, chan=None):
        e = self.eng[engname]
        waits = {}

        def need(tok):
            if tok is None:
                return
            s, v = tok
            if engname == "pe" and s == e.sem:
                return
            if waits.get(s, 0) < v:
                waits[s] = v

        reads = list(reads)
        writes = list(writes)
        xr = [a for a in reads if a.x]
        if xr:
            reads = [a for a in reads if not a.x]
            writes = writes + [a for a in xr if a not in writes]
        for a in reads:
            need(a.w)
        for a in writes:
            need(a.w)
            for s, v in a.r.items():
                need((s, v))
        wl = []
        for s, v in waits.items():
            if e.seen.get(s, 0) < v:
                e.seen[s] = v
                wl.append((s, v))
        if engname == "sp":
            chan.count += 16
            tok = (chan.sem, chan.count)
            incinfo = (chan.sem, 16)
        else:
            if inc:
                e.count += 1
                tok = (e.sem, e.count)
                incinfo = (e.sem, 1)
            else:
                tok = (e.sem, e.count + 1)
                incinfo = None
        e.ops.append((wl, fn, incinfo))
        for a in reads:
            if a.r.get(tok[0], 0) < tok[1]:
                a.r[tok[0]] = tok[1]
        for a in writes:
            a.w = tok
            a.r = {}

    def emit(self, engname, engine):
        e = self.eng[engname]
        for wl, fn, incinfo in e.ops:
            for s, v in wl:
                engine.wait_ge(self.sems[s], v)
            ins = fn(engine)
            if incinfo is not None:
                ins.then_inc(self.sems[incinfo[0]], incinfo[1])
        if engname == "sp":
            for c in self.chans:
                if c.count > 0:
                    engine.wait_ge(self.sems[c.sem], c.count)


def unit_list():
    u = []
    for c in range(8):
        u.append(("gv", c))
    for c in range(8):
        u.append(("gu", c))

    def branch(n):
        for dcp in range(8):
            u.append(("gate", n, 2 * dcp))
            u.append(("gate", n, 2 * dcp + 1))
            u.append(("br", n, dcp))

    branch(0)
    for c in range(8):
        u.append(("pz", c))
    u.append(("pw",))
    branch(1)
    for c in range(8):
        u.append(("q", c))
    for c in range(8):
        u.append(("k", c))
    for c in range(8):
        u.append(("av", c))
    branch(2)
    for dc in range(16):
        u.append(("wo", dc))
    for fg in range(4):
        for fc in range(16):
            u.append(("up", fg, fc))
        for dc in range(16):
            u.append(("dn", fg, dc))
    for dc in range(16):
        u.append(("pg", dc))
    return u


UNITS = unit_list()
UOFF = []
_o = 0
for _u in UNITS:
    UOFF.append(_o)
    _o += PGSZ if _u[0] == "pg" else USZ
WTOT = _o


def _kunit(W):
    K = W.shape[0]
    return np.ascontiguousarray(W.reshape(K // 128, 128, 128).transpose(1, 0, 2)).reshape(128, K)


def build_wstream(w_in, w_pool, w_branch, w_out, w_up, w_down, w_ple, w_ple_gate):
    out = np.empty((128, WTOT), dtype=np.float32)
    win_c = lambda c: w_in[:, c * 128:(c + 1) * 128]
    for ui, u in enumerate(UNITS):
        o = UOFF[ui]
        k = u[0]
        if k == "gv":
            a = _kunit(win_c(8 + u[1]))
        elif k == "gu":
            a = _kunit(win_c(u[1]))
        elif k == "gate":
            a = _kunit(win_c(48 + u[1] * 16 + u[2]))
        elif k == "br":
            n, dcp = u[1], u[2]
            a = np.concatenate([_kunit(w_branch[n][:, (2 * dcp + j) * 128:(2 * dcp + j + 1) * 128]) for j in range(2)], axis=1)
        elif k == "pz":
            a = _kunit(win_c(16 + u[1]))
        elif k == "pw":
            a = np.ascontiguousarray(w_pool.reshape(4, 2, 128, 256).transpose(2, 0, 1, 3)).reshape(128, 2048)
        elif k == "q":
            a = _kunit(win_c(24 + u[1]))
        elif k == "k":
            a = _kunit(win_c(32 + u[1]))
        elif k == "av":
            a = _kunit(win_c(40 + u[1]))
        elif k == "wo":
            a = _kunit(w_out[:, u[1] * 128:(u[1] + 1) * 128])
        elif k == "up":
            c = u[1] * 16 + u[2]
            a = _kunit(w_up[:, c * 128:(c + 1) * 128])
        elif k == "dn":
            fg, dc = u[1], u[2]
            a = _kunit(w_down[fg * 2048:(fg + 1) * 2048, dc * 128:(dc + 1) * 128])
        elif k == "pg":
            dc = u[1]
            a = np.concatenate([_kunit(w_ple_gate[:, dc * 128:(dc + 1) * 128]),
                                _kunit(w_ple[:, dc * 128:(dc + 1) * 128])], axis=1)
        out[:, o:o + a.shape[1]] = a
    return out


def build_consts():
    c = {}
    c["ident"] = np.eye(128, dtype=np.float32)
    c["ones"] = np.ones((128, 128), dtype=np.float32)
    ind = np.zeros((128, 64), dtype=np.float32)
    for k in range(128):
        ind[k, k % 64] = 1.0
    c["ind"] = ind
    s = np.arange(128)[:, None]
    t = np.arange(128)[None, :]
    tri = np.where(s <= t, 0.0, NEGM).astype(np.float32)
    cm = np.zeros((128, 2, 256), dtype=np.float32)
    cm[:, 0, 0:128] = tri
    cm[:, 0, 128:256] = 0.0
    cm[:, 1, 0:128] = NEGM
    cm[:, 1, 128:256] = tri
    c["cmask"] = cm
    slopes = np.exp2(-8.0 * np.arange(1, 9, dtype=np.float64) / 8.0)
    ak = np.zeros((128, 8, 16), dtype=np.float64)
    for idx in range(16):
        ak[:, :, idx] = (np.arange(128)[:, None] - (idx - 1) * 128) * slopes[None, :]
    c["akey"] = ak.astype(np.float32)
    pn = np.zeros((128, 8, 8), dtype=np.float32)
    no = np.ones((128, 8, 8), dtype=np.float32)
    for qb in range(8):
        for n in range(8):
            if n >= qb:
                pn[:, qb, n] = -1e30
            if n == qb:
                no[:, qb, n] = 0.0
    c["pastneg"] = pn
    c["notown"] = no
    aq = np.zeros((128, 2, 8, 8), dtype=np.float64)
    for par in range(2):
        aq[:, par, :, :] = (-(np.arange(128)[:, None] + 128 * par) * slopes[None, :])[:, :, None]
    c["aq"] = aq.reshape(128, 2, 64).astype(np.float32)
    c["spmask"] = np.where(s <= t, 1.0, 0.0).astype(np.float32)
    ic = np.zeros((128, 4, 16), dtype=np.float32)
    for g, w in enumerate((2, 4, 8, 16)):
        ic[:, g, :] = 1.0 / np.minimum(np.arange(1, 17), w)
    c["invcnt"] = ic
    return c


CONST_SHAPES = {
    "ident": [128, 128], "ones": [128, 128], "ind": [128, 64], "cmask": [128, 2, 256],
    "akey": [128, 8, 16], "pastneg": [128, 8, 8], "notown": [128, 8, 8], "aq": [128, 2, 64],
    "spmask": [128, 128], "invcnt": [128, 4, 16],
}
BF_CONSTS = ("ident", "ones", "ind", "cmask")
LCONST_SHAPES = {
    "ln1g": [128, 16], "ln1b": [128, 16], "ln2g": [128, 16], "ln2b": [128, 16],
    "pscale": [128, 8], "vng": [128, 1024], "vnb": [128, 1024], "bsp": [128, 4, 128],
    "wspT": [128, 4, 128],
}
L_VIA_TMP = ("vng", "vnb", "wspT")


def build_program(n_layers=DEPTH, n_tiles=NT, stop_stage=99):
    nc = bass.Bass("TRN2", target_bir_lowering=False)
    sc = Sched(nc)

    def dram(name, shape, kind):
        return nc.dram_tensor(name, shape, F32, kind=kind).ap()

    xT_d = dram("xT", [D, S], "ExternalInput")
    pT_d = dram("pT", [DEPTH, PLE, S], "ExternalInput")
    ws_d = dram("wstream", [DEPTH, 128, WTOT], "ExternalInput")
    cd = {k: dram("c_" + k, v, "ExternalInput") for k, v in CONST_SHAPES.items()}
    lcd = {k: dram("l_" + k, [DEPTH] + v, "ExternalInput") for k, v in LCONST_SHAPES.items()}
    out_d = dram("outT", [D, S], "ExternalOutput")
    scr_d = dram("xscr", [D, S], "Internal") if n_layers > 1 else None
    scr_atoms = [[Atom() for _ in range(16)] for _ in range(NT)]

    def sb(name, shape, dt, natoms=1):
        return Buf(nc.alloc_sbuf_tensor(name, shape, dt), natoms)

    def ps(name, natoms=1):
        b = Buf(nc.alloc_psum_tensor(name, [128, 512], F32), natoms)
        for a in b.atoms:
            a.x = True
        return b

    kT = sb("kT", [128, NH, S], BF16, 16)
    Vh = sb("Vh", [128, 16, MIXW], BF16, 16)
    kmean = sb("kmean", [128, NH, 8], BF16, 1)
    R = sb("R", [128, 16, TT], F32, 16)
    XB = [sb("XB0", [128, 16, TT], BF16, 16), sb("XB1", [128, 16, TT], BF16, 16)]
    MIXF = sb("MIXF", [128, 16, TT], BF16, 16)
    NSTG, NWB, NTMP = 3, 2, 6
    NW = TT + 16
    stg = [sb("stg%d" % i, [128, USZ], F32) for i in range(NSTG)]
    wbs = [sb("wb%d" % i, [128, PGSZ], BF16) for i in range(NWB)]
    tmps = [sb("tmp%d" % i, [128, NW], F32) for i in range(NTMP)]
    halo = sb("halo", [128, 8, 16], F32, 8)
    pTb = sb("pTb", [128, 2, TT], BF16)
    cT = sb("cT", [128, TT], BF16, 4)
    PTs = [sb("PT%d" % i, [128, 256], BF16) for i in range(3)]
    small = {k: sb("sm_" + k, shp, dt) for k, shp, dt in [
        ("gm", [128, 64], F32), ("max8", [128, 8, 8], F32), ("cf", [128, 64], F32),
        ("c2", [128, 64], F32), ("chl", [128, 128], BF16), ("ksum", [128, 2], F32),
        ("bst", [128, 2, 6], F32), ("mv", [128, 2], F32), ("rstd", [128, 1], F32),
    ]}
    cb = {}
    for k, shp in CONST_SHAPES.items():
        cb[k] = sb("cb_" + k, shp, BF16 if k in BF_CONSTS else F32)
    lcb = {}
    for k, shp in LCONST_SHAPES.items():
        if k == "wspT":
            continue
        lcb[k] = sb("lcb_" + k, shp, BF16 if k in L_VIA_TMP else F32)
    wspb = sb("wspb", [128, 4, 128], BF16)
    mmps = [ps("mm%d" % i) for i in range(4)]
    Sps = [ps("S%d" % i) for i in range(2)]
    OSps = [ps("OS%d" % i) for i in range(2)]

    ch_stg = [sc.chan("stg%d" % i) for i in range(NSTG)]
    ch_tmp = [sc.chan("tmp%d" % i) for i in range(NTMP)]
    ch_R = [sc.chan("R%d" % i) for i in range(16)]

    state = {"tmp": 0, "mm": 0, "stg": 0, "wb": 0, "S": 0, "PT": 0}

    def next_tmp():
        i = state["tmp"]
        state["tmp"] = (i + 1) % NTMP
        return tmps[i], ch_tmp[i]

    def T(tb):
        return tb.t[:, 0:TT]

    def TB(tb):
        return tb.t[:].bitcast(BF16)[:, 0:TT]

    def T4(tb):
        return tb.t[:, 0:TT].rearrange("p (a b) -> p a b", a=4)

    def next_mm():
        i = state["mm"]
        state["mm"] = (i + 1) % 4
        return mmps[i]

    def pe(fn, reads, writes, inc=False):
        sc.issue("pe", fn, reads, writes, inc=inc)

    def act(fn, reads, writes):
        sc.issue("act", fn, reads, writes)

    def dve(fn, reads, writes):
        sc.issue("dve", fn, reads, writes)

    def pool(fn, reads, writes):
        sc.issue("pool", fn, reads, writes)

    def dma(fn, reads, writes, chan):
        sc.issue("sp", fn, reads, writes, chan=chan)

    def flat(ap, nd):
        return ap.rearrange("p a b -> p (a b)") if nd == 3 else ap

    def load_via_tmp(dst_ap, dst_atoms, src_ap, n, mul_ap=None, mul_atoms=()):
        for o in range(0, n, TT):
            m = min(TT, n - o)
            tb, tch = next_tmp()
            dma(lambda e, tb=tb, o=o, m=m: e.dma_start(out=tb.t[:, 0:m], in_=src_ap[:, o:o + m]), [], tb.a(), tch)
            if mul_ap is None:
                dve(lambda e, tb=tb, o=o, m=m: e.tensor_copy(out=dst_ap[:, o:o + m], in_=tb.t[:, 0:m]), tb.a(), dst_atoms)
            else:
                dve(lambda e, tb=tb, o=o, m=m: e.tensor_tensor(out=dst_ap[:, o:o + m].rearrange("p (a b) -> p a b", a=4),
                                                               in0=tb.t[:, 0:m].rearrange("p (a b) -> p a b", a=4),
                                                               in1=mul_ap, op=ALU.mult), tb.a() + list(mul_atoms), dst_atoms)

    for k, shp in CONST_SHAPES.items():
        if k in BF_CONSTS:
            load_via_tmp(flat(cb[k].t[:], len(shp)), cb[k].a(), flat(cd[k], len(shp)), int(np.prod(shp[1:])))
        else:
            ch = sc.chan("cb_" + k)
            dma(lambda e, k=k: e.dma_start(out=cb[k].t[:], in_=cd[k]), [], cb[k].a(), ch)
    dve(lambda e: e.memset(kmean.t[:], 0.0), [], kmean.a())
    lchan = {k: sc.chan("lcb_" + k) for k in LCONST_SHAPES if k not in L_VIA_TMP}

    def load_unit(l, ui):
        is_pg = UNITS[ui][0] == "pg"
        off = UOFF[ui]
        si = state["stg"]
        state["stg"] = (si + 1) % NSTG
        wi = state["wb"]
        state["wb"] = (wi + 1) % NWB
        sg, wb = stg[si], wbs[wi]
        dma(lambda e: e.dma_start(out=sg.t[:, 0:USZ], in_=ws_d[l, :, off:off + USZ]), [], sg.a(), ch_stg[si])
        act(lambda e: e.activation(out=wb.t[:, 0:USZ], in_=sg.t[:, 0:USZ], func=AF.Identity), sg.a(), wb.a())
        if is_pg:
            tb, tch = next_tmp()
            dma(lambda e: e.dma_start(out=tb.t[:, 0:PGSZ - USZ], in_=ws_d[l, :, off + USZ:off + PGSZ]), [], tb.a(), tch)
            dve(lambda e: e.tensor_copy(out=wb.t[:, USZ:PGSZ], in_=tb.t[:, 0:PGSZ - USZ]), tb.a(), wb.a())
        return wb

    def mm_f1(wb, woff, nk, act_fn, act_atoms, psb):
        for kc in range(nk):
            pe(lambda e, kc=kc: e.matmul(psb.t[:], lhsT=wb.t[:, woff + kc * 128: woff + (kc + 1) * 128],
                                         rhs=act_fn(kc), start=(kc == 0), stop=(kc == nk - 1)),
               wb.a() + act_atoms, psb.a(), inc=(kc == nk - 1))

    def mm_f2(wb, xb, psb):
        for tc in range(4):
            for kc in range(16):
                pe(lambda e, tc=tc, kc=kc: e.matmul(psb.t[:, tc * 128:(tc + 1) * 128],
                                                    lhsT=xb.t[:, kc, tc * 128:(tc + 1) * 128],
                                                    rhs=wb.t[:, kc * 128:(kc + 1) * 128],
                                                    start=(kc == 0), stop=(kc == 15)),
                   wb.a() + xb.a(), psb.a(), inc=(tc == 3 and kc == 15))

    def gelu_from_psum(psb, out_ap_fn, out_atoms, view3=False):
        if view3:
            act(lambda e: e.activation(out=out_ap_fn(), in_=psb.t[:].rearrange("p (a b) -> p a b", a=4), func=AF.Gelu_apprx_tanh),
                psb.a(), out_atoms)
        else:
            act(lambda e: e.activation(out=out_ap_fn(), in_=psb.t[:], func=AF.Gelu_apprx_tanh), psb.a(), out_atoms)

    ones_bf = cb["ones"]

    def mixf_f32(i):
        return MIXF.t[:, 2 * i:2 * i + 2, :].rearrange("p a b -> p (a b)").bitcast(F32)

    def ln_stats_chunk(dc):
        ps_sum, ps_sq = Sps[0], Sps[1]
        t1, _ = next_tmp()
        act(lambda e, t1=t1: e.activation(out=TB(t1), in_=R.t[:, dc, :], func=AF.Identity), R.a(dc), t1.a())
        t2, _ = next_tmp()
        act(lambda e, t2=t2: e.activation(out=TB(t2), in_=R.t[:, dc, :], func=AF.Square), R.a(dc), t2.a())
        pe(lambda e, t1=t1: e.matmul(ps_sum.t[:], lhsT=ones_bf.t[:], rhs=TB(t1), start=(dc == 0), stop=(dc == 15)),
           t1.a() + ones_bf.a(), ps_sum.a(), inc=True)
        pe(lambda e, t2=t2: e.matmul(ps_sq.t[:], lhsT=ones_bf.t[:], rhs=TB(t2), start=(dc == 0), stop=(dc == 15)),
           t2.a() + ones_bf.a(), ps_sq.a(), inc=True)

    def layer_norm_R(gk, bk, xb_out):
        ps_sum, ps_sq = Sps[0], Sps[1]
        M, A, B = mixf_f32(0), mixf_f32(1), mixf_f32(2)
        Ma, Aa, Ba = MIXF.a(0, 2), MIXF.a(2, 4), MIXF.a(4, 6)
        dve(lambda e: e.tensor_scalar(out=M, in0=ps_sum.t[:], scalar1=1.0 / D, scalar2=None, op0=ALU.mult), ps_sum.a(), Ma)
        dve(lambda e: e.tensor_tensor(out=B, in0=M, in1=M, op=ALU.mult), Ma, Ba)
        dve(lambda e: e.scalar_tensor_tensor(out=A, in0=ps_sq.t[:], scalar=1.0 / D, in1=B, op0=ALU.mult, op1=ALU.subtract),
            ps_sq.a() + Ba, Aa)
        dve(lambda e: e.tensor_scalar(out=A, in0=A, scalar1=LN_EPS, scalar2=None, op0=ALU.add), Aa, Aa)
        act(lambda e: e.activation(out=A, in_=A, func=AF.Sqrt), Aa, Aa)
        dve(lambda e: e.reciprocal(out=A, in_=A), Aa, Aa)
        dve(lambda e: e.scalar_tensor_tensor(out=B, in0=M, scalar=-1.0, in1=A, op0=ALU.mult, op1=ALU.mult), Ma + Aa, Ba)
        for dc in range(16):
            t1, _ = next_tmp()
            dve(lambda e, dc=dc, t1=t1: e.tensor_tensor(out=T(t1), in0=R.t[:, dc, :], in1=A, op=ALU.mult), R.a(dc) + Aa, t1.a())
            dve(lambda e, t1=t1: e.tensor_tensor(out=T(t1), in0=T(t1), in1=B, op=ALU.add), t1.a() + Ba, t1.a())
            act(lambda e, dc=dc, t1=t1: e.activation(out=R.t[:, dc, :], in_=T(t1), func=AF.Identity,
                                                     scale=lcb[gk].t[:, dc:dc + 1], bias=lcb[bk].t[:, dc:dc + 1]),
                t1.a() + lcb[gk].a() + lcb[bk].a(), R.a(dc))
            if xb_out is not None:
                act(lambda e, dc=dc, t1=t1: e.activation(out=xb_out.t[:, dc, :], in_=T(t1), func=AF.Identity,
                                                         scale=lcb[gk].t[:, dc:dc + 1], bias=lcb[bk].t[:, dc:dc + 1]),
                    t1.a() + lcb[gk].a() + lcb[bk].a(), xb_out.a(dc))

    def load_xbf(l, ti, xb):
        src = xT_d if l == 0 else scr_d
        for dc in range(16):
            tb, tch = next_tmp()
            rd = [] if l == 0 else [scr_atoms[ti][dc]]
            dma(lambda e, dc=dc, tb=tb: e.dma_start(out=T(tb), in_=src[dc * 128:(dc + 1) * 128, ti * TT:(ti + 1) * TT]),
                rd, tb.a(), tch)
            dve(lambda e, dc=dc, tb=tb: e.tensor_copy(out=xb.t[:, dc, :], in_=T(tb)), tb.a(), xb.a(dc))

    def R_bf(c0, n):
        return R.t[:, c0:c0 + n, :].rearrange("p a b -> p (a b)").bitcast(BF16)

    unit_seq = [(l_, u_) for l_ in range(n_layers) for _t in range(n_tiles) for u_ in range(len(UNITS))]
    upos = [0]
    pending = []

    def nxt_global():
        if not pending:
            pending.append(load_unit(*unit_seq[upos[0]]))
        w = pending.pop(0)
        upos[0] += 1
        if upos[0] < len(unit_seq) and stop_stage == 99:
            pending.append(load_unit(*unit_seq[upos[0]]))
        return w

    for l in range(n_layers):
        for k in LCONST_SHAPES:
            if k in ("vng", "vnb"):
                load_via_tmp(lcb[k].t[:], lcb[k].a(), lcd[k][l], 1024)
            elif k == "wspT":
                load_via_tmp(wspb.t[:].rearrange("p a b -> p (a b)"), wspb.a(), lcd[k][l].rearrange("p a b -> p (a b)"), 512,
                             mul_ap=cb["spmask"].t[:].unsqueeze(1).to_broadcast([128, 4, 128]), mul_atoms=cb["spmask"].a())
            else:
                dma(lambda e, k=k, l=l: e.dma_start(out=lcb[k].t[:], in_=lcd[k][l]), [], lcb[k].a(), lchan[k])
        dve(lambda e: e.memset(halo.t[:], 0.0), [], halo.a())
        load_xbf(l, 0, XB[0])

        def tile_body(l, ti):
            xb = XB[0]
            xb1 = XB[1]
            ui = [0]

            def finish():
                for dc in range(16):
                    dma(lambda e, dc=dc: e.dma_start(out=out_d[dc * 128:(dc + 1) * 128, ti * TT:(ti + 1) * TT], in_=R.t[:, dc, :]),
                        R.a(dc), [], ch_R[dc])

            def nxt():
                assert unit_seq[upos[0]] == (l, ui[0])
                ui[0] += 1
                return nxt_global()

            xact = lambda kc: xb.t[:, kc, :]
            x1act = lambda kc: xb1.t[:, kc, :]
            mixact = lambda kc: MIXF.t[:, kc, :]

            for kc in range(2):
                tb, tch = next_tmp()
                dma(lambda e, kc=kc, tb=tb: e.dma_start(out=T(tb), in_=pT_d[l, kc * 128:(kc + 1) * 128, ti * TT:(ti + 1) * TT]),
                    [], tb.a(), tch)
                dve(lambda e, kc=kc, tb=tb: e.tensor_copy(out=pTb.t[:, kc, :], in_=T(tb)), tb.a(), pTb.a())

            vtok = R.t[:, 0:8, :].rearrange("p a b -> p (a b)").rearrange("p (t c) -> p t c", t=4)
            vbf = R_bf(8, 4).rearrange("p (t c) -> p t c", t=4)
            for c in range(8):
                wb = nxt()
                psb = next_mm()
                mm_f2(wb, xb, psb)
                gelu_from_psum(psb, lambda c=c: vtok[:, :, c * 128:(c + 1) * 128], R.a(0, 8), view3=True)
            bst, mv, rstd = small["bst"], small["mv"], small["rstd"]
            for tc in range(4):
                for hh in range(2):
                    dve(lambda e, tc=tc, hh=hh: e.bn_stats(out=bst.t[:, hh, :], in_=vtok[:, tc, hh * 512:(hh + 1) * 512]), R.a(0, 8), bst.a())
                dve(lambda e: e.bn_aggr(out=mv.t[:], in_=bst.t[:].rearrange("p a b -> p (a b)")), bst.a(), mv.a())
                dve(lambda e: e.tensor_scalar(out=rstd.t[:], in0=mv.t[:, 1:2], scalar1=LN_EPS, scalar2=None, op0=ALU.add), mv.a(), rstd.a())
                act(lambda e: e.activation(out=rstd.t[:], in_=rstd.t[:], func=AF.Sqrt), rstd.a(), rstd.a())
                dve(lambda e: e.reciprocal(out=rstd.t[:], in_=rstd.t[:]), rstd.a(), rstd.a())
                dve(lambda e, tc=tc: e.tensor_scalar(out=vtok[:, tc, :], in0=vtok[:, tc, :], scalar1=mv.t[:, 0:1], scalar2=rstd.t[:, 0:1],
                                                     op0=ALU.subtract, op1=ALU.mult), R.a(0, 8) + mv.a() + rstd.a(), R.a(0, 8))
                dve(lambda e, tc=tc: e.tensor_tensor(out=vtok[:, tc, :], in0=vtok[:, tc, :], in1=lcb["vng"].t[:], op=ALU.mult),
                    R.a(0, 8) + lcb["vng"].a(), R.a(0, 8))
                dve(lambda e, tc=tc: e.tensor_tensor(out=vbf[:, tc, :], in0=vtok[:, tc, :], in1=lcb["vnb"].t[:], op=ALU.add),
                    R.a(0, 8) + lcb["vnb"].a(), R.a(8, 12))

            if stop_stage == 1:
                finish()
                return
            ybf = R_bf(12, 4).rearrange("p (c t) -> p c t", c=8)
            Y_ATOMS = R.a(12, 16)
            yact = lambda kc: ybf[:, kc, :]

            for c in range(8):
                wb = nxt()
                psb = next_mm()
                mm_f1(wb, 0, 16, xact, xb.a(), psb)
                tu, _ = next_tmp()
                gelu_from_psum(psb, lambda tu=tu: T(tu), tu.a())
                ps2 = next_mm()
                g = c // 2
                for tc in range(4):
                    pe(lambda e, tc=tc, c=c, g=g, ps2=ps2: e.matmul(ps2.t[:, tc * 128:(tc + 1) * 128], lhsT=vbf[:, tc, c * 128:(c + 1) * 128],
                                                                    rhs=wspb.t[:, g, :], start=True, stop=True),
                       R.a(8, 12) + wspb.a(), ps2.a(), inc=(tc == 3))
                t3, _ = next_tmp()
                dve(lambda e, g=g, ps2=ps2, t3=t3: e.tensor_tensor(out=T4(t3), in0=ps2.t[:].rearrange("p (a b) -> p a b", a=4),
                                                                   in1=lcb["bsp"].t[:, g, :].unsqueeze(1).to_broadcast([128, 4, 128]), op=ALU.add),
                    ps2.a() + lcb["bsp"].a(), t3.a())
                dve(lambda e, c=c, t3=t3, tu=tu: e.tensor_tensor(out=ybf[:, c, :], in0=T(t3), in1=T(tu), op=ALU.mult),
                    t3.a() + tu.a(), Y_ATOMS)

            if stop_stage == 2:
                finish()
                return
            def do_branch(n):
                for dcp in range(8):
                    wg0 = nxt()
                    pg0 = next_mm()
                    mm_f1(wg0, 0, 16, xact, xb.a(), pg0)
                    sg0, _ = next_tmp()
                    act(lambda e, pg0=pg0, sg0=sg0: e.activation(out=T(sg0), in_=pg0.t[:], func=AF.Sigmoid), pg0.a(), sg0.a())
                    wg1 = nxt()
                    pg1 = next_mm()
                    mm_f1(wg1, 0, 16, xact, xb.a(), pg1)
                    sg1, _ = next_tmp()
                    act(lambda e, pg1=pg1, sg1=sg1: e.activation(out=T(sg1), in_=pg1.t[:], func=AF.Sigmoid), pg1.a(), sg1.a())
                    wbr = nxt()
                    for j, sg in ((0, sg0), (1, sg1)):
                        dc = 2 * dcp + j
                        pb = next_mm()
                        mm_f1(wbr, j * 1024, 8, yact, Y_ATOMS, pb)
                        if n == 0:
                            dve(lambda e, dc=dc, pb=pb, sg=sg: e.tensor_tensor(out=MIXF.t[:, dc, :], in0=pb.t[:], in1=T(sg), op=ALU.mult),
                                pb.a() + sg.a(), MIXF.a(dc))
                        else:
                            dve(lambda e, pb=pb, sg=sg: e.tensor_tensor(out=T(sg), in0=pb.t[:], in1=T(sg), op=ALU.mult),
                                pb.a() + sg.a(), sg.a())
                            dve(lambda e, dc=dc, sg=sg: e.tensor_tensor(out=MIXF.t[:, dc, :], in0=T(sg), in1=MIXF.t[:, dc, :], op=ALU.add),
                                sg.a() + MIXF.a(dc), MIXF.a(dc))

            do_branch(0)

            if stop_stage == 3:
                finish()
                return
            pooled = R_bf(0, 4).rearrange("p (c t) -> p c t", c=8)
            P_ATOMS = R.a(0, 4)
            for c in range(8):
                wb = nxt()
                psb = next_mm()
                mm_f1(wb, 0, 16, xact, xb.a(), psb)
                zc, _ = next_tmp()
                g = c // 2
                w = 2 << g
                act(lambda e, zc=zc, psb=psb: e.activation(out=zc.t[:, 16:NW], in_=psb.t[:], func=AF.Identity), psb.a(), zc.a())
                dve(lambda e, zc=zc, c=c: e.tensor_copy(out=zc.t[:, 0:16], in_=halo.t[:, c, :]), halo.a(c) + zc.a(), zc.a())
                src = zc
                sh = 1
                while sh < w:
                    dst, _ = next_tmp()
                    dve(lambda e, src=src, dst=dst, sh=sh: e.tensor_tensor(out=dst.t[:, sh:NW], in0=src.t[:, sh:NW], in1=src.t[:, 0:NW - sh], op=ALU.add),
                        src.a(), dst.a())
                    src = dst
                    sh *= 2
                dve(lambda e, src=src, zc=zc, c=c, w=w: e.scalar_tensor_tensor(out=pooled[:, c, :], in0=src.t[:, 16:NW], scalar=1.0 / w,
                                                                               in1=zc.t[:, 16:NW], op0=ALU.mult, op1=ALU.subtract),
                    src.a() + zc.a(), P_ATOMS)
                if ti == 0:
                    t1, _ = next_tmp()
                    dve(lambda e, src=src, g=g, t1=t1: e.tensor_tensor(out=t1.t[:, 0:16], in0=src.t[:, 16:32], in1=cb["invcnt"].t[:, g, :], op=ALU.mult),
                        src.a() + cb["invcnt"].a(), t1.a())
                    dve(lambda e, zc=zc, c=c, t1=t1: e.tensor_tensor(out=pooled[:, c, 0:16], in0=t1.t[:, 0:16], in1=zc.t[:, 16:32], op=ALU.subtract),
                        t1.a() + zc.a(), P_ATOMS)
                dve(lambda e, zc=zc, c=c: e.tensor_copy(out=halo.t[:, c, :], in_=zc.t[:, TT:NW]), zc.a(), halo.a(c))
            wb = nxt()
            for g in range(4):
                for hf in range(2):
                    pb = next_mm()
                    for kc in range(2):
                        o = g * 512 + kc * 256 + hf * 128
                        pe(lambda e, o=o, g=g, kc=kc, pb=pb, wb=wb: e.matmul(pb.t[:], lhsT=wb.t[:, o:o + 128], rhs=pooled[:, 2 * g + kc, :],
                                                                             start=(kc == 0), stop=(kc == 1)),
                           wb.a() + P_ATOMS, pb.a(), inc=(kc == 1))
                    cc = 2 * g + hf
                    dve(lambda e, cc=cc, pb=pb: e.tensor_scalar(out=ybf[:, cc, :], in0=pb.t[:], scalar1=lcb["pscale"].t[:, cc:cc + 1], scalar2=None, op0=ALU.mult),
                        pb.a() + lcb["pscale"].a(), Y_ATOMS)
            if stop_stage == 4:
                finish()
                return
            do_branch(1)

            qT = R_bf(0, 4).rearrange("p (c t) -> p c t", c=8)
            Q_ATOMS = R.a(0, 4)
            for h in range(8):
                wb = nxt()
                psb = next_mm()
                mm_f1(wb, 0, 16, xact, xb.a(), psb)
                act(lambda e, h=h, psb=psb: e.activation(out=qT[:, h, :], in_=psb.t[:], func=AF.Identity, scale=float(128 ** -0.5)), psb.a(), Q_ATOMS)
            ks = small["ksum"]
            for h in range(8):
                wb = nxt()
                psb = next_mm()
                mm_f1(wb, 0, 16, xact, xb.a(), psb)
                act(lambda e, h=h, psb=psb: e.activation(out=kT.t[:, h, ti * TT:(ti + 1) * TT], in_=psb.t[:], func=AF.Identity),
                    psb.a(), kT.a(4 * ti, 4 * ti + 4))
                dve(lambda e, psb=psb: e.tensor_reduce(out=ks.t[:], in_=psb.t[:].rearrange("p (a b) -> p a b", a=2), axis=AX.X, op=ALU.add), psb.a(), ks.a())
                dve(lambda e, h=h: e.tensor_scalar(out=kmean.t[:, h, 2 * ti:2 * ti + 2], in0=ks.t[:], scalar1=1.0 / 256, scalar2=None, op0=ALU.mult),
                    ks.a(), kmean.a())
            gm, max8, cf, c2 = small["gm"], small["max8"], small["cf"], small["c2"]
            chl4, _ = next_tmp()
            chlv = lambda tc: chl4.t[:].bitcast(BF16)[:, tc * 128:(tc + 1) * 128]
            v88 = lambda ap: ap.rearrange("p (a b) -> p a b", a=8)
            for tc in range(4):
                qblk = (ti * TT + tc * 128) // 256
                psg = next_mm()
                for h in range(8):
                    pe(lambda e, h=h, tc=tc, psg=psg: e.matmul(psg.t[:, h * 8:(h + 1) * 8], lhsT=qT[:, h, tc * 128:(tc + 1) * 128], rhs=kmean.t[:, h, :],
                                                               start=True, stop=True), Q_ATOMS + kmean.a(), psg.a(), inc=(h == 7))
                dve(lambda e, psg=psg, qblk=qblk: e.tensor_tensor(out=v88(gm.t[:]), in0=v88(psg.t[:, 0:64]),
                                                                  in1=cb["pastneg"].t[:, qblk, :].unsqueeze(1).to_broadcast([128, 8, 8]), op=ALU.add),
                    psg.a() + cb["pastneg"].a(), gm.a())
                for h in range(8):
                    dve(lambda e, h=h: e.max(out=max8.t[:, h, :], in_=gm.t[:, h * 8:(h + 1) * 8]), gm.a(), max8.a())
                for h in range(8):
                    dve(lambda e, h=h: e.tensor_scalar(out=cf.t[:, h * 8:(h + 1) * 8], in0=gm.t[:, h * 8:(h + 1) * 8], scalar1=max8.t[:, h, 2:3],
                                                       scalar2=NEGM, op0=ALU.is_lt, op1=ALU.mult), gm.a() + max8.a(), cf.a())
                dve(lambda e, qblk=qblk: e.tensor_tensor(out=v88(cf.t[:]), in0=v88(cf.t[:]),
                                                         in1=cb["notown"].t[:, qblk, :].unsqueeze(1).to_broadcast([128, 8, 8]), op=ALU.mult),
                    cf.a() + cb["notown"].a(), cf.a())
                dve(lambda e, tc=tc: e.tensor_tensor(out=c2.t[:], in0=cf.t[:], in1=cb["aq"].t[:, tc % 2, :], op=ALU.add), cf.a() + cb["aq"].a(), c2.a())
                dve(lambda e, tc=tc: e.tensor_copy(out=chlv(tc)[:, 0:64], in_=c2.t[:]), c2.a(), chl4.a())
                dve(lambda e, tc=tc: e.tensor_tensor(out=chlv(tc)[:, 64:128], in0=c2.t[:], in1=chlv(tc)[:, 0:64], op=ALU.subtract), c2.a() + chl4.a(), chl4.a())
            for h in range(8):
                wb = nxt()
                psb = next_mm()
                mm_f2(wb, xb, psb)
                act(lambda e, h=h, psb=psb: e.activation(out=Vh.t[:, 4 * ti:4 * ti + 4, h * 128:(h + 1) * 128],
                                                         in_=psb.t[:].rearrange("p (a b) -> p a b", a=4), func=AF.Identity),
                    psb.a(), Vh.a(4 * ti, 4 * ti + 4))

            if stop_stage == 5:
                finish()
                return
            for tc in range(4):
                pst = next_mm()
                pe(lambda e, pst=pst, tc=tc: e.transpose(out=pst.t[:].bitcast(BF16)[:, 0:128], in_=chlv(tc), identity=cb["ident"].t[:]),
                   chl4.a() + cb["ident"].a(), pst.a(), inc=True)
                act(lambda e, tc=tc, pst=pst: e.activation(out=cT.t[:, tc * 128:(tc + 1) * 128], in_=pst.t[:].bitcast(BF16)[:, 0:128], func=AF.Identity),
                    pst.a(), cT.a(tc))

            if stop_stage == 6:
                finish()
                return
            for qb in range(2):
                jq = 2 * ti + qb
                nkc = 2 * jq + 2
                qs = slice(qb * 256, (qb + 1) * 256)
                for h in range(8):
                    hp = h % 2
                    half = hp * 256
                    pend = None

                    OS = OSps[hp]

                    def pv(kc, PT, first, last, h=h, OS=OS):
                        pe(lambda e: e.matmul(OS.t[:, 0:256], lhsT=Vh.t[:, kc, h * 128:(h + 1) * 128], rhs=PT.t[:],
                                              start=first, stop=last, skip_group_check=True), Vh.a(kc) + PT.a(), OS.a(), inc=False)
                        pe(lambda e: e.matmul(OS.t[:, 256:512], lhsT=ones_bf.t[:], rhs=PT.t[:], start=False, stop=last, skip_group_check=True),
                           ones_bf.a() + PT.a(), OS.a(), inc=True)

                    for kc in range(nkc):
                        n = kc // 2
                        si = state["S"]
                        state["S"] = 1 - si
                        Sb = Sps[si]
                        own = (n == jq)
                        pe(lambda e, kc=kc, Sb=Sb, h=h, qs=qs: e.matmul(Sb.t[:, 0:256], lhsT=kT.t[:, h, kc * 128:(kc + 1) * 128], rhs=qT[:, h, qs],
                                                                 start=True, stop=False), kT.a(kc) + Q_ATOMS, Sb.a(), inc=False)
                        r = h * 8 + n
                        pe(lambda e, Sb=Sb, r=r, own=own, qs=qs: e.matmul(Sb.t[:, 0:256], lhsT=cb["ind"].t[:, r:r + 1].to_broadcast([128, 128]),
                                                                   rhs=cT.t[:, qs], start=False, stop=(not own)),
                           cb["ind"].a() + cT.a(2 * qb, 2 * qb + 2), Sb.a(), inc=(not own))
                        if own:
                            kcl = kc - 2 * jq
                            pe(lambda e, Sb=Sb, kcl=kcl: e.matmul(Sb.t[:, 0:256], lhsT=cb["ident"].t[:], rhs=cb["cmask"].t[:, kcl, :],
                                                                  start=False, stop=True), cb["ident"].a() + cb["cmask"].a(), Sb.a(), inc=True)
                        pi = state["PT"]
                        state["PT"] = (pi + 1) % 3
                        PT = PTs[pi]
                        didx = 2 * jq - kc + 1
                        act(lambda e, Sb=Sb, PT=PT, h=h, didx=didx: e.activation(out=PT.t[:], in_=Sb.t[:, 0:256], func=AF.Exp,
                                                                                 bias=cb["akey"].t[:, h, didx:didx + 1], scale=1.0),
                            Sb.a() + cb["akey"].a(), PT.a())
                        if pend is not None:
                            pv(*pend)
                        pend = (kc, PT, kc == 0, kc == nkc - 1)
                    pv(*pend)
                    rc, _ = next_tmp()
                    dve(lambda e, rc=rc, OS=OS: e.reciprocal(out=rc.t[:, 0:256], in_=OS.t[:, 256:512]), OS.a(), rc.a())
                    dve(lambda e, rc=rc, OS=OS, h=h, qs=qs: e.tensor_tensor(out=ybf[:, h, qs], in0=OS.t[:, 0:256], in1=rc.t[:, 0:256], op=ALU.mult),
                        OS.a() + rc.a(), Y_ATOMS)
            if stop_stage == 7:
                finish()
                return
            do_branch(2)

            if ti + 1 < n_tiles:
                load_xbf(l, ti + 1, XB[0])

            src_d = xT_d if l == 0 else scr_d
            for dc in range(16):
                rd = [] if l == 0 else [scr_atoms[ti][dc]]
                dma(lambda e, dc=dc, src_d=src_d: e.dma_start(out=R.t[:, dc, :], in_=src_d[dc * 128:(dc + 1) * 128, ti * TT:(ti + 1) * TT]),
                    rd, R.a(dc), ch_R[dc])

            for dc in range(16):
                wb = nxt()
                psb = next_mm()
                mm_f1(wb, 0, 16, mixact, MIXF.a(), psb)
                dve(lambda e, dc=dc, psb=psb: e.scalar_tensor_tensor(out=R.t[:, dc, :], in0=R.t[:, dc, :], scalar=ALPHA, in1=psb.t[:],
                                                                     op0=ALU.mult, op1=ALU.add), R.a(dc) + psb.a(), R.a(dc))
                ln_stats_chunk(dc)
            layer_norm_R("ln1g", "ln1b", xb1)

            if stop_stage == 8:
                finish()
                return
            for fg in range(4):
                for fc in range(16):
                    wb = nxt()
                    psb = next_mm()
                    mm_f1(wb, 0, 16, x1act, xb1.a(), psb)
                    t1, _ = next_tmp()
                    dve(lambda e, psb=psb, t1=t1: e.tensor_scalar(out=T(t1), in0=psb.t[:], scalar1=0.0, scalar2=None, op0=ALU.max), psb.a(), t1.a())
                    act(lambda e, fc=fc, t1=t1: e.activation(out=MIXF.t[:, fc, :], in_=T(t1), func=AF.Square), t1.a(), MIXF.a(fc))
                for dc in range(16):
                    wb = nxt()
                    psb = next_mm()
                    mm_f1(wb, 0, 16, mixact, MIXF.a(), psb)
                    if fg == 0:
                        dve(lambda e, dc=dc, psb=psb: e.scalar_tensor_tensor(out=R.t[:, dc, :], in0=R.t[:, dc, :], scalar=ALPHA, in1=psb.t[:],
                                                                             op0=ALU.mult, op1=ALU.add), R.a(dc) + psb.a(), R.a(dc))
                    else:
                        dve(lambda e, dc=dc, psb=psb: e.tensor_tensor(out=R.t[:, dc, :], in0=R.t[:, dc, :], in1=psb.t[:], op=ALU.add),
                            R.a(dc) + psb.a(), R.a(dc))
            for dc in range(16):
                wb = nxt()
                pg = next_mm()
                mm_f1(wb, 0, 16, x1act, xb1.a(), pg)
                pa = next_mm()
                mm_f1(wb, 2048, 2, lambda kc: pTb.t[:, kc, :], pTb.a(), pa)
                sg, _ = next_tmp()
                act(lambda e, pg=pg, sg=sg: e.activation(out=T(sg), in_=pg.t[:], func=AF.Sigmoid), pg.a(), sg.a())
                dve(lambda e, pa=pa, sg=sg: e.tensor_tensor(out=T(sg), in0=pa.t[:], in1=T(sg), op=ALU.mult), pa.a() + sg.a(), sg.a())
                dve(lambda e, dc=dc, sg=sg: e.tensor_tensor(out=R.t[:, dc, :], in0=R.t[:, dc, :], in1=T(sg), op=ALU.add), R.a(dc) + sg.a(), R.a(dc))
                ln_stats_chunk(dc)
            assert ui[0] == len(UNITS)
            layer_norm_R("ln2g", "ln2b", None)

            last = (l == n_layers - 1)
            dst_d = out_d if last else scr_d
            for dc in range(16):
                wr = [] if last else [scr_atoms[ti][dc]]
                dma(lambda e, dc=dc, dst_d=dst_d: e.dma_start(out=dst_d[dc * 128:(dc + 1) * 128, ti * TT:(ti + 1) * TT], in_=R.t[:, dc, :]),
                    R.a(dc), wr, ch_R[dc])

        for ti in range(n_tiles):
            tile_body(l, ti)

    for h in sc.sems:
        nc.gpsimd.sem_clear(h)
    nc.all_engine_barrier()
    with nc.Block() as block:
        @block.sync
        def _(e):
            sc.emit("sp", e)

        @block.tensor
        def _(e):
            sc.emit("pe", e)

        @block.scalar
        def _(e):
            sc.emit("act", e)

        @block.vector
        def _(e):
            sc.emit("dve", e)

        @block.gpsimd
        def _(e):
            sc.emit("pool", e)
    build_program.stats = {k: len(v.ops) for k, v in sc.eng.items()}
    build_program.sbuf_left = nc.sbuf_bytes_remaining
    for h in sc.sems:
        nc.gpsimd.sem_clear(h)
    nc.all_engine_barrier()
    return nc


def make_in_maps(x, p, w_in, w_sp, b_sp, vn_g, vn_b, w_pool, pool_scale, w_branch, w_out,
                 ln1_g, ln1_b, w_up, w_down, w_ple, w_ple_gate, ln2_g, ln2_b, cores=range(8)):
    f = lambda a: np.asarray(a, dtype=np.float32)
    x, p = f(x), f(p)
    ws = np.stack([build_wstream(f(w_in[l]), f(w_pool[l]), f(w_branch[l]), f(w_out[l]), f(w_up[l]), f(w_down[l]),
                                 f(w_ple[l]), f(w_ple_gate[l])) for l in range(DEPTH)])
    consts = build_consts()
    col = lambda v: np.ascontiguousarray(f(v).reshape(DEPTH, -1, 128).transpose(0, 2, 1))
    lc = {
        "ln1g": col(ln1_g), "ln1b": col(ln1_b), "ln2g": col(ln2_g), "ln2b": col(ln2_b),
        "pscale": col(pool_scale),
        "vng": np.ascontiguousarray(np.broadcast_to(f(vn_g)[:, None, :], (DEPTH, 128, MIXW))),
        "vnb": np.ascontiguousarray(np.broadcast_to(f(vn_b)[:, None, :], (DEPTH, 128, MIXW))),
        "bsp": np.ascontiguousarray(np.broadcast_to(f(b_sp)[:, None, :, :], (DEPTH, 128, 4, 128))),
        "wspT": np.ascontiguousarray(f(w_sp).transpose(0, 3, 1, 2)),
    }
    in_maps = []
    for b in cores:
        m = {"xT": np.ascontiguousarray(x[b].T), "pT": np.ascontiguousarray(p[:, b].transpose(0, 2, 1)), "wstream": ws}
        for k, v in consts.items():
            m["c_" + k] = v
        for k, v in lc.items():
            m["l_" + k] = v
        in_maps.append(m)
    return in_maps


_NC_CACHE = {}


def kernel(**inputs):
    in_maps = make_in_maps(**inputs)
    if "nc" not in _NC_CACHE:
        _NC_CACHE["nc"] = build_program()
    nc = _NC_CACHE["nc"]
    res = run_bass_kernel_spmd(nc, in_maps, core_ids=list(range(8)))
    out = np.stack([np.ascontiguousarray(r["outT"].T) for r in res.results], axis=0)
    return out.astype(np.float32)
```

```python
import numpy as np
import concourse.bass as bass
import concourse.mybir as mybir
from concourse.bass_utils import run_bass_kernel_spmd

F32 = mybir.dt.float32
BF16 = mybir.dt.bfloat16
AF = mybir.ActivationFunctionType
ALU = mybir.AluOpType
AX = mybir.AxisListType

D = 2048
S = 2048
DEPTH = 2
MIXW = 1024
DFF = 8192
PLE = 256
TT = 512
NT = S // TT
NH = 8
ALPHA = (2.0 * DEPTH) ** 0.25
LN_EPS = 1e-5
NEGM = -30000.0
USZ = 2048
PGSZ = 2304


class Atom:
    __slots__ = ("w", "r", "x")

    def __init__(self, x=False):
        self.w = None
        self.r = {}
        self.x = x


class Buf:
    def __init__(self, t, natoms=1):
        self.t = t
        self.atoms = [Atom() for _ in range(natoms)]

    def a(self, i=None, j=None):
        if i is None:
            return self.atoms
        if j is None:
            return [self.atoms[i]]
        return self.atoms[i:j]


class Chan:
    def __init__(self, sem):
        self.sem = sem
        self.count = 0


class EngState:
    def __init__(self, name, sem):
        self.name = name
        self.sem = sem
        self.count = 0
        self.seen = {}
        self.ops = []


class Sched:
    def __init__(self, nc):
        self.nc = nc
        self.sems = []
        self.eng = {}
        for n in ("pe", "act", "dve", "pool"):
            self.eng[n] = EngState(n, self.new_sem("s_" + n))
        self.eng["sp"] = EngState("sp", None)
        self.chans = []

    def new_sem(self, name):
        h = self.nc.alloc_semaphore(name)
        self.sems.append(h)
        return len(self.sems) - 1

    def chan(self, name):
        c = Chan(self.new_sem("c_" + name))
        self.chans.append(c)
        return c

    def issue(self, engname, fn, reads=(), writes=(), inc=True, chan=None):
        e = self.eng[engname]
        waits = {}

        def need(tok):
            if tok is None:
                return
            s, v = tok
            if engname == "pe" and s == e.sem:
                return
            if waits.get(s, 0) < v:
                waits[s] = v

        reads = list(reads)
        writes = list(writes)
        xr = [a for a in reads if a.x]
        if xr:
            reads = [a for a in reads if not a.x]
            writes = writes + [a for a in xr if a not in writes]
        for a in reads:
            need(a.w)
        for a in writes:
            need(a.w)
            for s, v in a.r.items():
                need((s, v))
        wl = []
        for s, v in waits.items():
            if e.seen.get(s, 0) < v:
                e.seen[s] = v
                wl.append((s, v))
        if engname == "sp":
            chan.count += 16
            tok = (chan.sem, chan.count)
            incinfo = (chan.sem, 16)
        else:
            if inc:
                e.count += 1
                tok = (e.sem, e.count)
                incinfo = (e.sem, 1)
            else:
                tok = (e.sem, e.count + 1)
                incinfo = None
        e.ops.append((wl, fn, incinfo))
        for a in reads:
            if a.r.get(tok[0], 0) < tok[1]:
                a.r[tok[0]] = tok[1]
        for a in writes:
            a.w = tok
            a.r = {}

    def emit(self, engname, engine):
        e = self.eng[engname]
        for wl, fn, incinfo in e.ops:
            for s, v in wl:
                engine.wait_ge(self.sems[s], v)
            ins = fn(engine)
            if incinfo is not None:
                ins.then_inc(self.sems[incinfo[0]], incinfo[1])
        if engname == "sp":
            for c in self.chans:
                if c.count > 0:
                    engine.wait_ge(self.sems[c.sem], c.count)


def unit_list():
    u = []
    for c in range(8):
        u.append(("gv", c))
    for c in range(8):
        u.append(("gu", c))

    def branch(n):
        for dcp in range(8):
            u.append(("gate", n, 2 * dcp))
            u.append(("gate", n, 2 * dcp + 1))
            u.append(("br", n, dcp))

    branch(0)
    for c in range(8):
        u.append(("pz", c))
    u.append(("pw",))
    branch(1)
    for c in range(8):
        u.append(("q", c))
    for c in range(8):
        u.append(("k", c))
    for c in range(8):
        u.append(("av", c))
    branch(2)
    for dc in range(16):
        u.append(("wo", dc))
    for fg in range(4):
        for fc in range(16):
            u.append(("up", fg, fc))
        for dc in range(16):
            u.append(("dn", fg, dc))
    for dc in range(16):
        u.append(("pg", dc))
    return u


UNITS = unit_list()
UOFF = []
_o = 0
for _u in UNITS:
    UOFF.append(_o)
    _o += PGSZ if _u[0] == "pg" else USZ
WTOT = _o


def _kunit(W):
    K = W.shape[0]
    return np.ascontiguousarray(W.reshape(K // 128, 128, 128).transpose(1, 0, 2)).reshape(128, K)


def build_wstream(w_in, w_pool, w_branch, w_out, w_up, w_down, w_ple, w_ple_gate):
    out = np.empty((128, WTOT), dtype=np.float32)
    win_c = lambda c: w_in[:, c * 128:(c + 1) * 128]
    for ui, u in enumerate(UNITS):
        o = UOFF[ui]
        k = u[0]
        if k == "gv":
            a = _kunit(win_c(8 + u[1]))
        elif k == "gu":
            a = _kunit(win_c(u[1]))
        elif k == "gate":
            a = _kunit(win_c(48 + u[1] * 16 + u[2]))
        elif k == "br":
            n, dcp = u[1], u[2]
            a = np.concatenate([_kunit(w_branch[n][:, (2 * dcp + j) * 128:(2 * dcp + j + 1) * 128]) for j in range(2)], axis=1)
        elif k == "pz":
            a = _kunit(win_c(16 + u[1]))
        elif k == "pw":
            a = np.ascontiguousarray(w_pool.reshape(4, 2, 128, 256).transpose(2, 0, 1, 3)).reshape(128, 2048)
        elif k == "q":
            a = _kunit(win_c(24 + u[1]))
        elif k == "k":
            a = _kunit(win_c(32 + u[1]))
        elif k == "av":
            a = _kunit(win_c(40 + u[1]))
        elif k == "wo":
            a = _kunit(w_out[:, u[1] * 128:(u[1] + 1) * 128])
        elif k == "up":
            c = u[1] * 16 + u[2]
            a = _kunit(w_up[:, c * 128:(c + 1) * 128])
        elif k == "dn":
            fg, dc = u[1], u[2]
            a = _kunit(w_down[fg * 2048:(fg + 1) * 2048, dc * 128:(dc + 1) * 128])
        elif k == "pg":
            dc = u[1]
            a = np.concatenate([_kunit(w_ple_gate[:, dc * 128:(dc + 1) * 128]),
                                _kunit(w_ple[:, dc * 128:(dc + 1) * 128])], axis=1)
        out[:, o:o + a.shape[1]] = a
    return out


def build_consts():
    c = {}
    c["ident"] = np.eye(128, dtype=np.float32)
    c["ones"] = np.ones((128, 128), dtype=np.float32)
    ind = np.zeros((128, 64), dtype=np.float32)
    for k in range(128):
        ind[k, k % 64] = 1.0
    c["ind"] = ind
    s = np.arange(128)[:, None]
    t = np.arange(128)[None, :]
    tri = np.where(s <= t, 0.0, NEGM).astype(np.float32)
    cm = np.zeros((128, 2, 256), dtype=np.float32)
    cm[:, 0, 0:128] = tri
    cm[:, 0, 128:256] = 0.0
    cm[:, 1, 0:128] = NEGM
    cm[:, 1, 128:256] = tri
    c["cmask"] = cm
    slopes = np.exp2(-8.0 * np.arange(1, 9, dtype=np.float64) / 8.0)
    ak = np.zeros((128, 8, 16), dtype=np.float64)
    for idx in range(16):
        ak[:, :, idx] = (np.arange(128)[:, None] - (idx - 1) * 128) * slopes[None, :]
    c["akey"] = ak.astype(np.float32)
    pn = np.zeros((128, 8, 8), dtype=np.float32)
    no = np.ones((128, 8, 8), dtype=np.float32)
    for qb in range(8):
        for n in range(8):
            if n >= qb:
                pn[:, qb, n] = -1e30
            if n == qb:
                no[:, qb, n] = 0.0
    c["pastneg"] = pn
    c["notown"] = no
    aq = np.zeros((128, 2, 8, 8), dtype=np.float64)
    for par in range(2):
        aq[:, par, :, :] = (-(np.arange(128)[:, None] + 128 * par) * slopes[None, :])[:, :, None]
    c["aq"] = aq.reshape(128, 2, 64).astype(np.float32)
    c["spmask"] = np.where(s <= t, 1.0, 0.0).astype(np.float32)
    ic = np.zeros((128, 4, 16), dtype=np.float32)
    for g, w in enumerate((2, 4, 8, 16)):
        ic[:, g, :] = 1.0 / np.minimum(np.arange(1, 17), w)
    c["invcnt"] = ic
    return c


CONST_SHAPES = {
    "ident": [128, 128], "ones": [128, 128], "ind": [128, 64], "cmask": [128, 2, 256],
    "akey": [128, 8, 16], "pastneg": [128, 8, 8], "notown": [128, 8, 8], "aq": [128, 2, 64],
    "spmask": [128, 128], "invcnt": [128, 4, 16],
}
BF_CONSTS = ("ident", "ones", "ind", "cmask")
LCONST_SHAPES = {
    "ln1g": [128, 16], "ln1b": [128, 16], "ln2g": [128, 16], "ln2b": [128, 16],
    "pscale": [128, 8], "vng": [128, 1024], "vnb": [128, 1024], "bsp": [128, 4, 128],
    "wspT": [128, 4, 128],
}
L_VIA_TMP = ("vng", "vnb", "wspT")


def build_program(n_layers=DEPTH, n_tiles=NT, stop_stage=99):
    nc = bass.Bass("TRN2", target_bir_lowering=False)
    sc = Sched(nc)

    def dram(name, shape, kind):
        return nc.dram_tensor(name, shape, F32, kind=kind).ap()

    xT_d = dram("xT", [D, S], "ExternalInput")
    pT_d = dram("pT", [DEPTH, PLE, S], "ExternalInput")
    ws_d = dram("wstream", [DEPTH, 128, WTOT], "ExternalInput")
    cd = {k: dram("c_" + k, v, "ExternalInput") for k, v in CONST_SHAPES.items()}
    lcd = {k: dram("l_" + k, [DEPTH] + v, "ExternalInput") for k, v in LCONST_SHAPES.items()}
    out_d = dram("outT", [D, S], "ExternalOutput")
    scr_d = dram("xscr", [D, S], "Internal") if n_layers > 1 else None
    scr_atoms = [[Atom() for _ in range(16)] for _ in range(NT)]

    def sb(name, shape, dt, natoms=1):
        return Buf(nc.alloc_sbuf_tensor(name, shape, dt), natoms)

    def ps(name, natoms=1):
        b = Buf(nc.alloc_psum_tensor(name, [128, 512], F32), natoms)
        for a in b.atoms:
            a.x = True
        return b

    kT = sb("kT", [128, NH, S], BF16, 16)
    Vh = sb("Vh", [128, 16, MIXW], BF16, 16)
    kmean = sb("kmean", [128, NH, 8], BF16, 1)
    R = sb("R", [128, 16, TT], F32, 16)
    XB = [sb("XB0", [128, 16, TT], BF16, 16), sb("XB1", [128, 16, TT], BF16, 16)]
    MIXF = sb("MIXF", [128, 16, TT], BF16, 16)
    NSTG, NWB, NTMP = 3, 2, 6
    NW = TT + 16
    stg = [sb("stg%d" % i, [128, USZ], F32) for i in range(NSTG)]
    wbs = [sb("wb%d" % i, [128, PGSZ], BF16) for i in range(NWB)]
    tmps = [sb("tmp%d" % i, [128, NW], F32) for i in range(NTMP)]
    halo = sb("halo", [128, 8, 16], F32, 8)
    pTb = sb("pTb", [128, 2, TT], BF16)
    cT = sb("cT", [128, TT], BF16, 4)
    PTs = [sb("PT%d" % i, [128, 256], BF16) for i in range(3)]
    small = {k: sb("sm_" + k, shp, dt) for k, shp, dt in [
        ("gm", [128, 64], F32), ("max8", [128, 8, 8], F32), ("cf", [128, 64], F32),
        ("c2", [128, 64], F32), ("chl", [128, 128], BF16), ("ksum", [128, 2], F32),
        ("bst", [128, 2, 6], F32), ("mv", [128, 2], F32), ("rstd", [128, 1], F32),
    ]}
    cb = {}
    for k, shp in CONST_SHAPES.items():
        cb[k] = sb("cb_" + k, shp, BF16 if k in BF_CONSTS else F32)
    lcb = {}
    for k, shp in LCONST_SHAPES.items():
        if k == "wspT":
            continue
        lcb[k] = sb("lcb_" + k, shp, BF16 if k in L_VIA_TMP else F32)
    wspb = sb("wspb", [128, 4, 128], BF16)
    mmps = [ps("mm%d" % i) for i in range(4)]
    Sps = [ps("S%d" % i) for i in range(2)]
    OSps = [ps("OS%d" % i) for i in range(2)]

    ch_stg = [sc.chan("stg%d" % i) for i in range(NSTG)]
    ch_tmp = [sc.chan("tmp%d" % i) for i in range(NTMP)]
    ch_R = [sc.chan("R%d" % i) for i in range(16)]

    state = {"tmp": 0, "mm": 0, "stg": 0, "wb": 0, "S": 0, "PT": 0}

    def next_tmp():
        i = state["tmp"]
        state["tmp"] = (i + 1) % NTMP
        return tmps[i], ch_tmp[i]

    def T(tb):
        return tb.t[:, 0:TT]

    def TB(tb):
        return tb.t[:].bitcast(BF16)[:, 0:TT]

    def T4(tb):
        return tb.t[:, 0:TT].rearrange("p (a b) -> p a b", a=4)

    def next_mm():
        i = state["mm"]
        state["mm"] = (i + 1) % 4
        return mmps[i]

    def pe(fn, reads, writes, inc=False):
        sc.issue("pe", fn, reads, writes, inc=inc)

    def act(fn, reads, writes):
        sc.issue("act", fn, reads, writes)

    def dve(fn, reads, writes):
        sc.issue("dve", fn, reads, writes)

    def pool(fn, reads, writes):
        sc.issue("pool", fn, reads, writes)

    def dma(fn, reads, writes, chan):
        sc.issue("sp", fn, reads, writes, chan=chan)

    def flat(ap, nd):
        return ap.rearrange("p a b -> p (a b)") if nd == 3 else ap

    def load_via_tmp(dst_ap, dst_atoms, src_ap, n, mul_ap=None, mul_atoms=()):
        for o in range(0, n, TT):
            m = min(TT, n - o)
            tb, tch = next_tmp()
            dma(lambda e, tb=tb, o=o, m=m: e.dma_start(out=tb.t[:, 0:m], in_=src_ap[:, o:o + m]), [], tb.a(), tch)
            if mul_ap is None:
                dve(lambda e, tb=tb, o=o, m=m: e.tensor_copy(out=dst_ap[:, o:o + m], in_=tb.t[:, 0:m]), tb.a(), dst_atoms)
            else:
                dve(lambda e, tb=tb, o=o, m=m: e.tensor_tensor(out=dst_ap[:, o:o + m].rearrange("p (a b) -> p a b", a=4),
                                                               in0=tb.t[:, 0:m].rearrange("p (a b) -> p a b", a=4),
                                                               in1=mul_ap, op=ALU.mult), tb.a() + list(mul_atoms), dst_atoms)

    for k, shp in CONST_SHAPES.items():
        if k in BF_CONSTS:
            load_via_tmp(flat(cb[k].t[:], len(shp)), cb[k].a(), flat(cd[k], len(shp)), int(np.prod(shp[1:])))
        else:
            ch = sc.chan("cb_" + k)
            dma(lambda e, k=k: e.dma_start(out=cb[k].t[:], in_=cd[k]), [], cb[k].a(), ch)
    dve(lambda e: e.memset(kmean.t[:], 0.0), [], kmean.a())
    lchan = {k: sc.chan("lcb_" + k) for k in LCONST_SHAPES if k not in L_VIA_TMP}

    def load_unit(l, ui):
        is_pg = UNITS[ui][0] == "pg"
        off = UOFF[ui]
        si = state["stg"]
        state["stg"] = (si + 1) % NSTG
        wi = state["wb"]
        state["wb"] = (wi + 1) % NWB
        sg, wb = stg[si], wbs[wi]
        dma(lambda e: e.dma_start(out=sg.t[:, 0:USZ], in_=ws_d[l, :, off:off + USZ]), [], sg.a(), ch_stg[si])
        act(lambda e: e.activation(out=wb.t[:, 0:USZ], in_=sg.t[:, 0:USZ], func=AF.Identity), sg.a(), wb.a())
        if is_pg:
            tb, tch = next_tmp()
            dma(lambda e: e.dma_start(out=tb.t[:, 0:PGSZ - USZ], in_=ws_d[l, :, off + USZ:off + PGSZ]), [], tb.a(), tch)
            dve(lambda e: e.tensor_copy(out=wb.t[:, USZ:PGSZ], in_=tb.t[:, 0:PGSZ - USZ]), tb.a(), wb.a())
        return wb

    def mm_f1(wb, woff, nk, act_fn, act_atoms, psb):
        for kc in range(nk):
            pe(lambda e, kc=kc: e.matmul(psb.t[:], lhsT=wb.t[:, woff + kc * 128: woff + (kc + 1) * 128],
                                         rhs=act_fn(kc), start=(kc == 0), stop=(kc == nk - 1)),
               wb.a() + act_atoms, psb.a(), inc=(kc == nk - 1))

    def mm_f2(wb, xb, psb):
        for tc in range(4):
            for kc in range(16):
                pe(lambda e, tc=tc, kc=kc: e.matmul(psb.t[:, tc * 128:(tc + 1) * 128],
                                                    lhsT=xb.t[:, kc, tc * 128:(tc + 1) * 128],
                                                    rhs=wb.t[:, kc * 128:(kc + 1) * 128],
                                                    start=(kc == 0), stop=(kc == 15)),
                   wb.a() + xb.a(), psb.a(), inc=(tc == 3 and kc == 15))

    def gelu_from_psum(psb, out_ap_fn, out_atoms, view3=False):
        if view3:
            act(lambda e: e.activation(out=out_ap_fn(), in_=psb.t[:].rearrange("p (a b) -> p a b", a=4), func=AF.Gelu_apprx_tanh),
                psb.a(), out_atoms)
        else:
            act(lambda e: e.activation(out=out_ap_fn(), in_=psb.t[:], func=AF.Gelu_apprx_tanh), psb.a(), out_atoms)

    ones_bf = cb["ones"]

    def mixf_f32(i):
        return MIXF.t[:, 2 * i:2 * i + 2, :].rearrange("p a b -> p (a b)").bitcast(F32)

    def ln_stats_prep(dc):
        t1, _ = next_tmp()
        act(lambda e, t1=t1: e.activation(out=TB(t1), in_=R.t[:, dc, :], func=AF.Identity), R.a(dc), t1.a())
        t2, _ = next_tmp()
        act(lambda e, t2=t2: e.activation(out=TB(t2), in_=R.t[:, dc, :], func=AF.Square), R.a(dc), t2.a())
        return (dc, t1, t2)

    def ln_stats_mm(prep):
        if prep is None:
            return
        dc, t1, t2 = prep
        ps_sum, ps_sq = Sps[0], Sps[1]
        pe(lambda e: e.matmul(ps_sum.t[:], lhsT=ones_bf.t[:], rhs=TB(t1), start=(dc == 0), stop=(dc == 15)),
           t1.a() + ones_bf.a(), ps_sum.a(), inc=True)
        pe(lambda e: e.matmul(ps_sq.t[:], lhsT=ones_bf.t[:], rhs=TB(t2), start=(dc == 0), stop=(dc == 15)),
           t2.a() + ones_bf.a(), ps_sq.a(), inc=True)

    def layer_norm_R(gk, bk, xb_out):
        ps_sum, ps_sq = Sps[0], Sps[1]
        M, A, B = mixf_f32(0), mixf_f32(1), mixf_f32(2)
        Ma, Aa, Ba = MIXF.a(0, 2), MIXF.a(2, 4), MIXF.a(4, 6)
        dve(lambda e: e.tensor_scalar(out=M, in0=ps_sum.t[:], scalar1=1.0 / D, scalar2=None, op0=ALU.mult), ps_sum.a(), Ma)
        dve(lambda e: e.tensor_tensor(out=B, in0=M, in1=M, op=ALU.mult), Ma, Ba)
        dve(lambda e: e.scalar_tensor_tensor(out=A, in0=ps_sq.t[:], scalar=1.0 / D, in1=B, op0=ALU.mult, op1=ALU.subtract),
            ps_sq.a() + Ba, Aa)
        dve(lambda e: e.tensor_scalar(out=A, in0=A, scalar1=LN_EPS, scalar2=None, op0=ALU.add), Aa, Aa)
        act(lambda e: e.activation(out=A, in_=A, func=AF.Sqrt), Aa, Aa)
        dve(lambda e: e.reciprocal(out=A, in_=A), Aa, Aa)
        dve(lambda e: e.scalar_tensor_tensor(out=B, in0=M, scalar=-1.0, in1=A, op0=ALU.mult, op1=ALU.mult), Ma + Aa, Ba)
        for dc in range(16):
            t1, _ = next_tmp()
            dve(lambda e, dc=dc, t1=t1: e.tensor_tensor(out=T(t1), in0=R.t[:, dc, :], in1=A, op=ALU.mult), R.a(dc) + Aa, t1.a())
            dve(lambda e, t1=t1: e.tensor_tensor(out=T(t1), in0=T(t1), in1=B, op=ALU.add), t1.a() + Ba, t1.a())
            act(lambda e, dc=dc, t1=t1: e.activation(out=R.t[:, dc, :], in_=T(t1), func=AF.Identity,
                                                     scale=lcb[gk].t[:, dc:dc + 1], bias=lcb[bk].t[:, dc:dc + 1]),
                t1.a() + lcb[gk].a() + lcb[bk].a(), R.a(dc))
            if xb_out is not None:
                act(lambda e, dc=dc, t1=t1: e.activation(out=xb_out.t[:, dc, :], in_=T(t1), func=AF.Identity,
                                                         scale=lcb[gk].t[:, dc:dc + 1], bias=lcb[bk].t[:, dc:dc + 1]),
                    t1.a() + lcb[gk].a() + lcb[bk].a(), xb_out.a(dc))

    def load_xbf(l, ti, xb):
        src = xT_d if l == 0 else scr_d
        for dc in range(16):
            tb, tch = next_tmp()
            rd = [] if l == 0 else [scr_atoms[ti][dc]]
            dma(lambda e, dc=dc, tb=tb: e.dma_start(out=T(tb), in_=src[dc * 128:(dc + 1) * 128, ti * TT:(ti + 1) * TT]),
                rd, tb.a(), tch)
            dve(lambda e, dc=dc, tb=tb: e.tensor_copy(out=xb.t[:, dc, :], in_=T(tb)), tb.a(), xb.a(dc))

    def R_bf(c0, n):
        return R.t[:, c0:c0 + n, :].rearrange("p a b -> p (a b)").bitcast(BF16)

    unit_seq = [(l_, u_) for l_ in range(n_layers) for _t in range(n_tiles) for u_ in range(len(UNITS))]
    upos = [0]
    pending = []

    def nxt_global():
        if not pending:
            pending.append(load_unit(*unit_seq[upos[0]]))
        w = pending.pop(0)
        upos[0] += 1
        if upos[0] < len(unit_seq) and stop_stage == 99:
            pending.append(load_unit(*unit_seq[upos[0]]))
        return w

    for l in range(n_layers):
        for k in LCONST_SHAPES:
            if k in ("vng", "vnb"):
                load_via_tmp(lcb[k].t[:], lcb[k].a(), lcd[k][l], 1024)
            elif k == "wspT":
                load_via_tmp(wspb.t[:].rearrange("p a b -> p (a b)"), wspb.a(), lcd[k][l].rearrange("p a b -> p (a b)"), 512,
                             mul_ap=cb["spmask"].t[:].unsqueeze(1).to_broadcast([128, 4, 128]), mul_atoms=cb["spmask"].a())
            else:
                dma(lambda e, k=k, l=l: e.dma_start(out=lcb[k].t[:], in_=lcd[k][l]), [], lcb[k].a(), lchan[k])
        dve(lambda e: e.memset(halo.t[:], 0.0), [], halo.a())
        load_xbf(l, 0, XB[0])

        def tile_body(l, ti):
            xb = XB[0]
            xb1 = XB[1]
            ui = [0]

            def finish():
                for dc in range(16):
                    dma(lambda e, dc=dc: e.dma_start(out=out_d[dc * 128:(dc + 1) * 128, ti * TT:(ti + 1) * TT], in_=R.t[:, dc, :]),
                        R.a(dc), [], ch_R[dc])

            def nxt():
                assert unit_seq[upos[0]] == (l, ui[0])
                ui[0] += 1
                return nxt_global()

            xact = lambda kc: xb.t[:, kc, :]
            x1act = lambda kc: xb1.t[:, kc, :]
            mixact = lambda kc: MIXF.t[:, kc, :]

            for kc in range(2):
                tb, tch = next_tmp()
                dma(lambda e, kc=kc, tb=tb: e.dma_start(out=T(tb), in_=pT_d[l, kc * 128:(kc + 1) * 128, ti * TT:(ti + 1) * TT]),
                    [], tb.a(), tch)
                dve(lambda e, kc=kc, tb=tb: e.tensor_copy(out=pTb.t[:, kc, :], in_=T(tb)), tb.a(), pTb.a())

            vtok = R.t[:, 0:8, :].rearrange("p a b -> p (a b)").rearrange("p (t c) -> p t c", t=4)
            vbf = R_bf(8, 4).rearrange("p (t c) -> p t c", t=4)
            for c in range(8):
                wb = nxt()
                psb = next_mm()
                mm_f2(wb, xb, psb)
                gelu_from_psum(psb, lambda c=c: vtok[:, :, c * 128:(c + 1) * 128], R.a(0, 8), view3=True)
            bst, mv, rstd = small["bst"], small["mv"], small["rstd"]
            for tc in range(4):
                for hh in range(2):
                    dve(lambda e, tc=tc, hh=hh: e.bn_stats(out=bst.t[:, hh, :], in_=vtok[:, tc, hh * 512:(hh + 1) * 512]), R.a(0, 8), bst.a())
                dve(lambda e: e.bn_aggr(out=mv.t[:], in_=bst.t[:].rearrange("p a b -> p (a b)")), bst.a(), mv.a())
                dve(lambda e: e.tensor_scalar(out=rstd.t[:], in0=mv.t[:, 1:2], scalar1=LN_EPS, scalar2=None, op0=ALU.add), mv.a(), rstd.a())
                act(lambda e: e.activation(out=rstd.t[:], in_=rstd.t[:], func=AF.Sqrt), rstd.a(), rstd.a())
                dve(lambda e: e.reciprocal(out=rstd.t[:], in_=rstd.t[:]), rstd.a(), rstd.a())
                dve(lambda e, tc=tc: e.tensor_scalar(out=vtok[:, tc, :], in0=vtok[:, tc, :], scalar1=mv.t[:, 0:1], scalar2=rstd.t[:, 0:1],
                                                     op0=ALU.subtract, op1=ALU.mult), R.a(0, 8) + mv.a() + rstd.a(), R.a(0, 8))
                dve(lambda e, tc=tc: e.tensor_tensor(out=vtok[:, tc, :], in0=vtok[:, tc, :], in1=lcb["vng"].t[:], op=ALU.mult),
                    R.a(0, 8) + lcb["vng"].a(), R.a(0, 8))
                dve(lambda e, tc=tc: e.tensor_tensor(out=vbf[:, tc, :], in0=vtok[:, tc, :], in1=lcb["vnb"].t[:], op=ALU.add),
                    R.a(0, 8) + lcb["vnb"].a(), R.a(8, 12))

            if stop_stage == 1:
                finish()
                return
            ybf = R_bf(12, 4).rearrange("p (c t) -> p c t", c=8)
            Y_ATOMS = R.a(12, 16)
            yact = lambda kc: ybf[:, kc, :]

            for c in range(8):
                wb = nxt()
                psb = next_mm()
                mm_f1(wb, 0, 16, xact, xb.a(), psb)
                tu, _ = next_tmp()
                gelu_from_psum(psb, lambda tu=tu: T(tu), tu.a())
                ps2 = next_mm()
                g = c // 2
                for tc in range(4):
                    pe(lambda e, tc=tc, c=c, g=g, ps2=ps2: e.matmul(ps2.t[:, tc * 128:(tc + 1) * 128], lhsT=vbf[:, tc, c * 128:(c + 1) * 128],
                                                                    rhs=wspb.t[:, g, :], start=True, stop=True),
                       R.a(8, 12) + wspb.a(), ps2.a(), inc=(tc == 3))
                t3, _ = next_tmp()
                dve(lambda e, g=g, ps2=ps2, t3=t3: e.tensor_tensor(out=T4(t3), in0=ps2.t[:].rearrange("p (a b) -> p a b", a=4),
                                                                   in1=lcb["bsp"].t[:, g, :].unsqueeze(1).to_broadcast([128, 4, 128]), op=ALU.add),
                    ps2.a() + lcb["bsp"].a(), t3.a())
                dve(lambda e, c=c, t3=t3, tu=tu: e.tensor_tensor(out=ybf[:, c, :], in0=T(t3), in1=T(tu), op=ALU.mult),
                    t3.a() + tu.a(), Y_ATOMS)

            if stop_stage == 2:
                finish()
                return
            def do_branch(n):
                for dcp in range(8):
                    wg0 = nxt()
                    pg0 = next_mm()
                    mm_f1(wg0, 0, 16, xact, xb.a(), pg0)
                    sg0, _ = next_tmp()
                    act(lambda e, pg0=pg0, sg0=sg0: e.activation(out=T(sg0), in_=pg0.t[:], func=AF.Sigmoid), pg0.a(), sg0.a())
                    wg1 = nxt()
                    pg1 = next_mm()
                    mm_f1(wg1, 0, 16, xact, xb.a(), pg1)
                    sg1, _ = next_tmp()
                    act(lambda e, pg1=pg1, sg1=sg1: e.activation(out=T(sg1), in_=pg1.t[:], func=AF.Sigmoid), pg1.a(), sg1.a())
                    wbr = nxt()
                    for j, sg in ((0, sg0), (1, sg1)):
                        dc = 2 * dcp + j
                        pb = next_mm()
                        mm_f1(wbr, j * 1024, 8, yact, Y_ATOMS, pb)
                        if n == 0:
                            dve(lambda e, dc=dc, pb=pb, sg=sg: e.tensor_tensor(out=MIXF.t[:, dc, :], in0=pb.t[:], in1=T(sg), op=ALU.mult),
                                pb.a() + sg.a(), MIXF.a(dc))
                        else:
                            dve(lambda e, pb=pb, sg=sg: e.tensor_tensor(out=T(sg), in0=pb.t[:], in1=T(sg), op=ALU.mult),
                                pb.a() + sg.a(), sg.a())
                            dve(lambda e, dc=dc, sg=sg: e.tensor_tensor(out=MIXF.t[:, dc, :], in0=T(sg), in1=MIXF.t[:, dc, :], op=ALU.add),
                                sg.a() + MIXF.a(dc), MIXF.a(dc))

            do_branch(0)

            if stop_stage == 3:
                finish()
                return
            pooled = R_bf(0, 4).rearrange("p (c t) -> p c t", c=8)
            P_ATOMS = R.a(0, 4)
            for c in range(8):
                wb = nxt()
                psb = next_mm()
                mm_f1(wb, 0, 16, xact, xb.a(), psb)
                zc, _ = next_tmp()
                g = c // 2
                w = 2 << g
                act(lambda e, zc=zc, psb=psb: e.activation(out=zc.t[:, 16:NW], in_=psb.t[:], func=AF.Identity), psb.a(), zc.a())
                dve(lambda e, zc=zc, c=c: e.tensor_copy(out=zc.t[:, 0:16], in_=halo.t[:, c, :]), halo.a(c) + zc.a(), zc.a())
                src = zc
                sh = 1
                while sh < w:
                    dst, _ = next_tmp()
                    dve(lambda e, src=src, dst=dst, sh=sh: e.tensor_tensor(out=dst.t[:, sh:NW], in0=src.t[:, sh:NW], in1=src.t[:, 0:NW - sh], op=ALU.add),
                        src.a(), dst.a())
                    src = dst
                    sh *= 2
                dve(lambda e, src=src, zc=zc, c=c, w=w: e.scalar_tensor_tensor(out=pooled[:, c, :], in0=src.t[:, 16:NW], scalar=1.0 / w,
                                                                               in1=zc.t[:, 16:NW], op0=ALU.mult, op1=ALU.subtract),
                    src.a() + zc.a(), P_ATOMS)
                if ti == 0:
                    t1, _ = next_tmp()
                    dve(lambda e, src=src, g=g, t1=t1: e.tensor_tensor(out=t1.t[:, 0:16], in0=src.t[:, 16:32], in1=cb["invcnt"].t[:, g, :], op=ALU.mult),
                        src.a() + cb["invcnt"].a(), t1.a())
                    dve(lambda e, zc=zc, c=c, t1=t1: e.tensor_tensor(out=pooled[:, c, 0:16], in0=t1.t[:, 0:16], in1=zc.t[:, 16:32], op=ALU.subtract),
                        t1.a() + zc.a(), P_ATOMS)
                dve(lambda e, zc=zc, c=c: e.tensor_copy(out=halo.t[:, c, :], in_=zc.t[:, TT:NW]), zc.a(), halo.a(c))
            wb = nxt()
            for g in range(4):
                for hf in range(2):
                    pb = next_mm()
                    for kc in range(2):
                        o = g * 512 + kc * 256 + hf * 128
                        pe(lambda e, o=o, g=g, kc=kc, pb=pb, wb=wb: e.matmul(pb.t[:], lhsT=wb.t[:, o:o + 128], rhs=pooled[:, 2 * g + kc, :],
                                                                             start=(kc == 0), stop=(kc == 1)),
                           wb.a() + P_ATOMS, pb.a(), inc=(kc == 1))
                    cc = 2 * g + hf
                    dve(lambda e, cc=cc, pb=pb: e.tensor_scalar(out=ybf[:, cc, :], in0=pb.t[:], scalar1=lcb["pscale"].t[:, cc:cc + 1], scalar2=None, op0=ALU.mult),
                        pb.a() + lcb["pscale"].a(), Y_ATOMS)
            if stop_stage == 4:
                finish()
                return
            do_branch(1)

            qT = R_bf(0, 4).rearrange("p (c t) -> p c t", c=8)
            Q_ATOMS = R.a(0, 4)
            for h in range(8):
                wb = nxt()
                psb = next_mm()
                mm_f1(wb, 0, 16, xact, xb.a(), psb)
                act(lambda e, h=h, psb=psb: e.activation(out=qT[:, h, :], in_=psb.t[:], func=AF.Identity, scale=float(128 ** -0.5)), psb.a(), Q_ATOMS)
            ks = small["ksum"]
            for h in range(8):
                wb = nxt()
                psb = next_mm()
                mm_f1(wb, 0, 16, xact, xb.a(), psb)
                act(lambda e, h=h, psb=psb: e.activation(out=kT.t[:, h, ti * TT:(ti + 1) * TT], in_=psb.t[:], func=AF.Identity),
                    psb.a(), kT.a(4 * ti, 4 * ti + 4))
                dve(lambda e, psb=psb: e.tensor_reduce(out=ks.t[:], in_=psb.t[:].rearrange("p (a b) -> p a b", a=2), axis=AX.X, op=ALU.add), psb.a(), ks.a())
                dve(lambda e, h=h: e.tensor_scalar(out=kmean.t[:, h, 2 * ti:2 * ti + 2], in0=ks.t[:], scalar1=1.0 / 256, scalar2=None, op0=ALU.mult),
                    ks.a(), kmean.a())
            gm, max8, cf, c2 = small["gm"], small["max8"], small["cf"], small["c2"]
            chl4, _ = next_tmp()
            chlv = lambda tc: chl4.t[:].bitcast(BF16)[:, tc * 128:(tc + 1) * 128]
            v88 = lambda ap: ap.rearrange("p (a b) -> p a b", a=8)
            for tc in range(4):
                qblk = (ti * TT + tc * 128) // 256
                psg = next_mm()
                for h in range(8):
                    pe(lambda e, h=h, tc=tc, psg=psg: e.matmul(psg.t[:, h * 8:(h + 1) * 8], lhsT=qT[:, h, tc * 128:(tc + 1) * 128], rhs=kmean.t[:, h, :],
                                                               start=True, stop=True), Q_ATOMS + kmean.a(), psg.a(), inc=(h == 7))
                dve(lambda e, psg=psg, qblk=qblk: e.tensor_tensor(out=v88(gm.t[:]), in0=v88(psg.t[:, 0:64]),
                                                                  in1=cb["pastneg"].t[:, qblk, :].unsqueeze(1).to_broadcast([128, 8, 8]), op=ALU.add),
                    psg.a() + cb["pastneg"].a(), gm.a())
                for h in range(8):
                    dve(lambda e, h=h: e.max(out=max8.t[:, h, :], in_=gm.t[:, h * 8:(h + 1) * 8]), gm.a(), max8.a())
                for h in range(8):
                    dve(lambda e, h=h: e.tensor_scalar(out=cf.t[:, h * 8:(h + 1) * 8], in0=gm.t[:, h * 8:(h + 1) * 8], scalar1=max8.t[:, h, 2:3],
                                                       scalar2=NEGM, op0=ALU.is_lt, op1=ALU.mult), gm.a() + max8.a(), cf.a())
                dve(lambda e, qblk=qblk: e.tensor_tensor(out=v88(cf.t[:]), in0=v88(cf.t[:]),
                                                         in1=cb["notown"].t[:, qblk, :].unsqueeze(1).to_broadcast([128, 8, 8]), op=ALU.mult),
                    cf.a() + cb["notown"].a(), cf.a())
                dve(lambda e, tc=tc: e.tensor_tensor(out=c2.t[:], in0=cf.t[:], in1=cb["aq"].t[:, tc % 2, :], op=ALU.add), cf.a() + cb["aq"].a(), c2.a())
                dve(lambda e, tc=tc: e.tensor_copy(out=chlv(tc)[:, 0:64], in_=c2.t[:]), c2.a(), chl4.a())
                dve(lambda e, tc=tc: e.tensor_tensor(out=chlv(tc)[:, 64:128], in0=c2.t[:], in1=chlv(tc)[:, 0:64], op=ALU.subtract), c2.a() + chl4.a(), chl4.a())
            for h in range(8):
                wb = nxt()
                psb = next_mm()
                mm_f2(wb, xb, psb)
                act(lambda e, h=h, psb=psb: e.activation(out=Vh.t[:, 4 * ti:4 * ti + 4, h * 128:(h + 1) * 128],
                                                         in_=psb.t[:].rearrange("p (a b) -> p a b", a=4), func=AF.Identity),
                    psb.a(), Vh.a(4 * ti, 4 * ti + 4))

            if stop_stage == 5:
                finish()
                return
            for tc in range(4):
                pst = next_mm()
                pe(lambda e, pst=pst, tc=tc: e.transpose(out=pst.t[:].bitcast(BF16)[:, 0:128], in_=chlv(tc), identity=cb["ident"].t[:]),
                   chl4.a() + cb["ident"].a(), pst.a(), inc=True)
                act(lambda e, tc=tc, pst=pst: e.activation(out=cT.t[:, tc * 128:(tc + 1) * 128], in_=pst.t[:].bitcast(BF16)[:, 0:128], func=AF.Identity),
                    pst.a(), cT.a(tc))

            if stop_stage == 6:
                finish()
                return
            for qb in range(2):
                jq = 2 * ti + qb
                nkc = 2 * jq + 2
                qs = slice(qb * 256, (qb + 1) * 256)
                for h in range(8):
                    hp = h % 2
                    half = hp * 256
                    pend = None

                    OS = OSps[hp]

                    def pv(kc, PT, first, last, h=h, OS=OS):
                        pe(lambda e: e.matmul(OS.t[:, 0:256], lhsT=Vh.t[:, kc, h * 128:(h + 1) * 128], rhs=PT.t[:],
                                              start=first, stop=last, skip_group_check=True), Vh.a(kc) + PT.a(), OS.a(), inc=False)
                        pe(lambda e: e.matmul(OS.t[:, 256:512], lhsT=ones_bf.t[:], rhs=PT.t[:], start=False, stop=last, skip_group_check=True),
                           ones_bf.a() + PT.a(), OS.a(), inc=True)

                    for kc in range(nkc):
                        n = kc // 2
                        si = state["S"]
                        state["S"] = 1 - si
                        Sb = Sps[si]
                        own = (n == jq)
                        pe(lambda e, kc=kc, Sb=Sb, h=h, qs=qs: e.matmul(Sb.t[:, 0:256], lhsT=kT.t[:, h, kc * 128:(kc + 1) * 128], rhs=qT[:, h, qs],
                                                                 start=True, stop=False), kT.a(kc) + Q_ATOMS, Sb.a(), inc=False)
                        r = h * 8 + n
                        pe(lambda e, Sb=Sb, r=r, own=own, qs=qs: e.matmul(Sb.t[:, 0:256], lhsT=cb["ind"].t[:, r:r + 1].to_broadcast([128, 128]),
                                                                   rhs=cT.t[:, qs], start=False, stop=(not own)),
                           cb["ind"].a() + cT.a(2 * qb, 2 * qb + 2), Sb.a(), inc=(not own))
                        if own:
                            kcl = kc - 2 * jq
                            pe(lambda e, Sb=Sb, kcl=kcl: e.matmul(Sb.t[:, 0:256], lhsT=cb["ident"].t[:], rhs=cb["cmask"].t[:, kcl, :],
                                                                  start=False, stop=True), cb["ident"].a() + cb["cmask"].a(), Sb.a(), inc=True)
                        pi = state["PT"]
                        state["PT"] = (pi + 1) % 3
                        PT = PTs[pi]
                        didx = 2 * jq - kc + 1
                        act(lambda e, Sb=Sb, PT=PT, h=h, didx=didx: e.activation(out=PT.t[:], in_=Sb.t[:, 0:256], func=AF.Exp,
                                                                                 bias=cb["akey"].t[:, h, didx:didx + 1], scale=1.0),
                            Sb.a() + cb["akey"].a(), PT.a())
                        if pend is not None:
                            pv(*pend)
                        pend = (kc, PT, kc == 0, kc == nkc - 1)
                    pv(*pend)
                    rc, _ = next_tmp()
                    dve(lambda e, rc=rc, OS=OS: e.reciprocal(out=rc.t[:, 0:256], in_=OS.t[:, 256:512]), OS.a(), rc.a())
                    dve(lambda e, rc=rc, OS=OS, h=h, qs=qs: e.tensor_tensor(out=ybf[:, h, qs], in0=OS.t[:, 0:256], in1=rc.t[:, 0:256], op=ALU.mult),
                        OS.a() + rc.a(), Y_ATOMS)
            if stop_stage == 7:
                finish()
                return
            do_branch(2)

            if ti + 1 < n_tiles:
                load_xbf(l, ti + 1, XB[0])

            src_d = xT_d if l == 0 else scr_d
            for dc in range(16):
                rd = [] if l == 0 else [scr_atoms[ti][dc]]
                dma(lambda e, dc=dc, src_d=src_d: e.dma_start(out=R.t[:, dc, :], in_=src_d[dc * 128:(dc + 1) * 128, ti * TT:(ti + 1) * TT]),
                    rd, R.a(dc), ch_R[dc])

            prep = None
            for dc in range(16):
                wb = nxt()
                psb = next_mm()
                mm_f1(wb, 0, 16, mixact, MIXF.a(), psb)
                ln_stats_mm(prep)
                dve(lambda e, dc=dc, psb=psb: e.scalar_tensor_tensor(out=R.t[:, dc, :], in0=R.t[:, dc, :], scalar=ALPHA, in1=psb.t[:],
                                                                     op0=ALU.mult, op1=ALU.add), R.a(dc) + psb.a(), R.a(dc))
                prep = ln_stats_prep(dc)
            ln_stats_mm(prep)
            layer_norm_R("ln1g", "ln1b", xb1)

            if stop_stage == 8:
                finish()
                return
            for fg in range(4):
                for fc in range(16):
                    wb = nxt()
                    psb = next_mm()
                    mm_f1(wb, 0, 16, x1act, xb1.a(), psb)
                    t1, _ = next_tmp()
                    dve(lambda e, psb=psb, t1=t1: e.tensor_scalar(out=T(t1), in0=psb.t[:], scalar1=0.0, scalar2=None, op0=ALU.max), psb.a(), t1.a())
                    act(lambda e, fc=fc, t1=t1: e.activation(out=MIXF.t[:, fc, :], in_=T(t1), func=AF.Square), t1.a(), MIXF.a(fc))
                for dc in range(16):
                    wb = nxt()
                    psb = next_mm()
                    mm_f1(wb, 0, 16, mixact, MIXF.a(), psb)
                    if fg == 0:
                        dve(lambda e, dc=dc, psb=psb: e.scalar_tensor_tensor(out=R.t[:, dc, :], in0=R.t[:, dc, :], scalar=ALPHA, in1=psb.t[:],
                                                                             op0=ALU.mult, op1=ALU.add), R.a(dc) + psb.a(), R.a(dc))
                    else:
                        dve(lambda e, dc=dc, psb=psb: e.tensor_tensor(out=R.t[:, dc, :], in0=R.t[:, dc, :], in1=psb.t[:], op=ALU.add),
                            R.a(dc) + psb.a(), R.a(dc))
            prep = None
            for dc in range(16):
                wb = nxt()
                pg = next_mm()
                mm_f1(wb, 0, 16, x1act, xb1.a(), pg)
                pa = next_mm()
                mm_f1(wb, 2048, 2, lambda kc: pTb.t[:, kc, :], pTb.a(), pa)
                ln_stats_mm(prep)
                sg, _ = next_tmp()
                act(lambda e, pg=pg, sg=sg: e.activation(out=T(sg), in_=pg.t[:], func=AF.Sigmoid), pg.a(), sg.a())
                dve(lambda e, pa=pa, sg=sg: e.tensor_tensor(out=T(sg), in0=pa.t[:], in1=T(sg), op=ALU.mult), pa.a() + sg.a(), sg.a())
                dve(lambda e, dc=dc, sg=sg: e.tensor_tensor(out=R.t[:, dc, :], in0=R.t[:, dc, :], in1=T(sg), op=ALU.add), R.a(dc) + sg.a(), R.a(dc))
                prep = ln_stats_prep(dc)
            ln_stats_mm(prep)
            assert ui[0] == len(UNITS)
            layer_norm_R("ln2g", "ln2b", None)

            last = (l == n_layers - 1)
            dst_d = out_d if last else scr_d
            for dc in range(16):
                wr = [] if last else [scr_atoms[ti][dc]]
                dma(lambda e, dc=dc, dst_d=dst_d: e.dma_start(out=dst_d[dc * 128:(dc + 1) * 128, ti * TT:(ti + 1) * TT], in_=R.t[:, dc, :]),
                    R.a(dc), wr, ch_R[dc])

        for ti in range(n_tiles):
            tile_body(l, ti)

    for h in sc.sems:
        nc.gpsimd.sem_clear(h)
    nc.all_engine_barrier()
    with nc.Block() as block:
        @block.sync
        def _(e):
            sc.emit("sp", e)

        @block.tensor
        def _(e):
            sc.emit("pe", e)

        @block.scalar
        def _(e):
            sc.emit("act", e)

        @block.vector
        def _(e):
            sc.emit("dve", e)

        @block.gpsimd
        def _(e):
            sc.emit("pool", e)
    build_program.stats = {k: len(v.ops) for k, v in sc.eng.items()}
    build_program.sbuf_left = nc.sbuf_bytes_remaining
    for h in sc.sems:
        nc.gpsimd.sem_clear(h)
    nc.all_engine_barrier()
    return nc


def make_in_maps(x, p, w_in, w_sp, b_sp, vn_g, vn_b, w_pool, pool_scale, w_branch, w_out,
                 ln1_g, ln1_b, w_up, w_down, w_ple, w_ple_gate, ln2_g, ln2_b, cores=range(8)):
    f = lambda a: np.asarray(a, dtype=np.float32)
    x, p = f(x), f(p)
    ws = np.stack([build_wstream(f(w_in[l]), f(w_pool[l]), f(w_branch[l]), f(w_out[l]), f(w_up[l]), f(w_down[l]),
                                 f(w_ple[l]), f(w_ple_gate[l])) for l in range(DEPTH)])
    consts = build_consts()
    col = lambda v: np.ascontiguousarray(f(v).reshape(DEPTH, -1, 128).transpose(0, 2, 1))
    lc = {
        "ln1g": col(ln1_g), "ln1b": col(ln1_b), "ln2g": col(ln2_g), "ln2b": col(ln2_b),
        "pscale": col(pool_scale),
        "vng": np.ascontiguousarray(np.broadcast_to(f(vn_g)[:, None, :], (DEPTH, 128, MIXW))),
        "vnb": np.ascontiguousarray(np.broadcast_to(f(vn_b)[:, None, :], (DEPTH, 128, MIXW))),
        "bsp": np.ascontiguousarray(np.broadcast_to(f(b_sp)[:, None, :, :], (DEPTH, 128, 4, 128))),
        "wspT": np.ascontiguousarray(f(w_sp).transpose(0, 3, 1, 2)),
    }
    in_maps = []
    for b in cores:
        m = {"xT": np.ascontiguousarray(x[b].T), "pT": np.ascontiguousarray(p[:, b].transpose(0, 2, 1)), "wstream": ws}
        for k, v in consts.items():
            m["c_" + k] = v
        for k, v in lc.items():
            m["l_" + k] = v
        in_maps.append(m)
    return in_maps


_NC_CACHE = {}


def kernel(**inputs):
    in_maps = make_in_maps(**inputs)
    if "nc" not in _NC_CACHE:
        _NC_CACHE["nc"] = build_program()
    nc = _NC_CACHE["nc"]
    res = run_bass_kernel_spmd(nc, in_maps, core_ids=list(range(8)))
    out = np.stack([np.ascontiguousarray(r["outT"].T) for r in res.results], axis=0)
    return out.astype(np.float32)
```
